# Optimizing a Trainium2 kernel written in Bass

```python
import jax, jax.numpy as jnp
from jax import lax
import numpy as np

D_MODEL = 1024
BATCH = 4
SEQ = 4096
DEPTH = 1

FOX_HEADS = 8
FOX_HEAD_DIM = 64
FOX_WIDTH = FOX_HEADS * FOX_HEAD_DIM
Q_BLOCK = 128
RET_HEADS = 8
RET_QK_DIM = 64
RET_V_DIM = 128
RET_QK_WIDTH = RET_HEADS * RET_QK_DIM
RET_V_WIDTH = RET_HEADS * RET_V_DIM
RET_CHUNK = 128
ROPE_BASE = 10000.0
SPLIT_SIZES = (FOX_WIDTH, FOX_WIDTH, FOX_WIDTH, FOX_HEADS,
               RET_QK_WIDTH, RET_QK_WIDTH, RET_V_WIDTH, RET_V_WIDTH,
               D_MODEL, D_MODEL)
IN_WIDTH = 3 * FOX_WIDTH + FOX_HEADS + 2 * RET_QK_WIDTH + 2 * RET_V_WIDTH + 2 * D_MODEL
N_EXPERTS = 256
TOP_K = 8
N_GROUPS = 8
TOPK_GROUPS = 4
EXPERT_DIM = 256
SHARED_DIM = 256
ROUTED_SCALE = 2.5
EXPERT_BLOCK = 128
NORM_EPS = 1e-6

kernel_name = "fox_retnet_griffin_merge_dsv3_moe_adaln"


def rms_norm(x, g):
    xf = x.astype(jnp.float32)
    y = xf * lax.rsqrt(jnp.mean(xf * xf, axis=-1, keepdims=True) + NORM_EPS)
    return (y * g.astype(jnp.float32)).astype(x.dtype)


def forgetting_attention(q, k, v, log_f):
    b, s, h, dh = q.shape
    q = q.transpose(0, 2, 1, 3)
    k = k.transpose(0, 2, 1, 3)
    v = v.transpose(0, 2, 1, 3)
    cum_f = jnp.cumsum(log_f, axis=1).transpose(0, 2, 1)
    scale = dh ** -0.5
    outs = []
    for i in range(s // Q_BLOCK):
        q0, q1 = i * Q_BLOCK, (i + 1) * Q_BLOCK
        qb = q[:, :, q0:q1]
        kp, vp = k[:, :, :q1], v[:, :, :q1]
        logits = jnp.einsum('bhqd,bhkd->bhqk', qb, kp).astype(jnp.float32) * scale
        logits = logits + cum_f[:, :, q0:q1, None] - cum_f[:, :, None, :q1]
        causal = jnp.arange(q1)[None, :] <= (q0 + jnp.arange(Q_BLOCK))[:, None]
        logits = jnp.where(causal, logits, -jnp.inf)
        p = jax.nn.softmax(logits, axis=-1)
        outs.append(jnp.einsum('bhqk,bhkd->bhqd', p.astype(vp.dtype), vp))
    o = jnp.concatenate(outs, axis=2)
    return o.transpose(0, 2, 1, 3).reshape(b, s, h * dh)


def rotary(x, pos):
    half = x.shape[-1] // 2
    inv_freq = ROPE_BASE ** (-jnp.arange(half, dtype=jnp.float32) / half)
    ang = pos.astype(jnp.float32)[:, None] * inv_freq[None, :]
    cos, sin = jnp.cos(ang)[None, :, None, :], jnp.sin(ang)[None, :, None, :]
    x1, x2 = x[..., :half], x[..., half:]
    return jnp.concatenate([x1 * cos - x2 * sin, x1 * sin + x2 * cos], axis=-1)


def chunkwise_retention(q, k, v):
    b, s, h, dk = q.shape
    dv = v.shape[-1]
    n = s // RET_CHUNK
    c = RET_CHUNK
    log_gamma = jnp.log1p(-jnp.exp2(-5.0 - jnp.arange(h, dtype=jnp.float32)))
    to_chunks = lambda t: t.reshape(b, n, c, h, t.shape[-1]).transpose(0, 3, 1, 2, 4)
    qc, kc, vc = to_chunks(q), to_chunks(k * dk ** -0.5), to_chunks(v)
    idx = jnp.arange(c, dtype=jnp.float32)
    diff = idx[:, None] - idx[None, :]
    intra_decay = jnp.where(diff >= 0,
                            jnp.exp(jnp.maximum(diff, 0.0)[None] * log_gamma[:, None, None]),
                            0.0)
    scores = jnp.einsum('bhnid,bhnjd->bhnij', qc, kc) * intra_decay[None, :, None]
    intra = jnp.einsum('bhnij,bhnje->bhnie', scores, vc)
    zeta = jnp.exp((c - 1.0 - idx)[None, :] * log_gamma[:, None])
    xi = jnp.exp((idx + 1.0)[None, :] * log_gamma[:, None])
    chunk_decay = jnp.exp(c * log_gamma)[:, None, None]
    kv = jnp.einsum('bhncd,bhnce->bhnde', kc * zeta[None, :, None, :, None], vc)
    kv = jnp.moveaxis(kv, 2, 0)

    def step(state, kv_n):
        return chunk_decay * state + kv_n, state

    _, prev = lax.scan(step, jnp.zeros((b, h, dk, dv), jnp.float32), kv)
    prev = jnp.moveaxis(prev, 0, 2)
    cross = jnp.einsum('bhncd,bhnde->bhnce', qc, prev) * xi[None, :, None, :, None]
    o = intra + cross
    return o.transpose(0, 2, 3, 1, 4).reshape(b, s, h, dv)


def mixing_sublayer(h, w_in, b_forget, ret_gn_g, w_branch_a, w_branch_b, w_out):
    b, s, _ = h.shape
    proj = jnp.einsum('bsd,de->bse', h, w_in)
    fq, fk, fv, ff, rq, rk, rv, rg, ga, gb = jnp.split(
        proj, list(np.cumsum(SPLIT_SIZES)[:-1]), axis=-1)
    heads_a = lambda t: t.reshape(b, s, FOX_HEADS, FOX_HEAD_DIM)
    log_f = jax.nn.log_sigmoid(ff.astype(jnp.float32) + b_forget.astype(jnp.float32))
    y_a = forgetting_attention(heads_a(fq), heads_a(fk), heads_a(fv), log_f)
    y_a = jnp.einsum('bse,ed->bsd', y_a, w_branch_a)
    pos = jnp.arange(s)
    rq = rotary(rq.astype(jnp.float32).reshape(b, s, RET_HEADS, RET_QK_DIM), pos)
    rk = rotary(rk.astype(jnp.float32).reshape(b, s, RET_HEADS, RET_QK_DIM), pos)
    rv = rv.astype(jnp.float32).reshape(b, s, RET_HEADS, RET_V_DIM)
    o = chunkwise_retention(rq, rk, rv)
    mu = jnp.mean(o, axis=-1, keepdims=True)
    var = jnp.mean(jnp.square(o - mu), axis=-1, keepdims=True)
    o = ((o - mu) * lax.rsqrt(var + NORM_EPS)).reshape(b, s, RET_V_WIDTH)
    o = (o * ret_gn_g.astype(jnp.float32)).astype(h.dtype) * jax.nn.silu(rg)
    y_b = jnp.einsum('bse,ed->bsd', o, w_branch_b)
    merged = jax.nn.sigmoid(ga) * y_a + jax.nn.sigmoid(gb) * y_b
    return jnp.einsum('bsd,de->bse', merged, w_out)


def swiglu(x, wg, wu, wd):
    return (jax.nn.silu(x @ wg) * (x @ wu)) @ wd


def moe_sublayer(h, w_router, router_bias, w_exp_gate, w_exp_up, w_exp_down,
                 w_sh_gate, w_sh_up, w_sh_down):
    b, s, d = h.shape
    hf = h.reshape(b * s, d)
    t = hf.shape[0]
    scores = jax.nn.sigmoid((hf @ w_router).astype(jnp.float32))
    sel = scores + router_bias.astype(jnp.float32)
    grp = sel.reshape(t, N_GROUPS, N_EXPERTS // N_GROUPS)
    group_scores = lax.top_k(grp, 2)[0].sum(-1)
    top_groups = lax.top_k(group_scores, TOPK_GROUPS)[1]
    group_mask = jax.nn.one_hot(top_groups, N_GROUPS, dtype=jnp.float32).sum(-2) > 0
    expert_mask = jnp.repeat(group_mask, N_EXPERTS // N_GROUPS, axis=-1)
    _, top_e = lax.top_k(jnp.where(expert_mask, sel, -jnp.inf), TOP_K)
    top_w = jnp.take_along_axis(scores, top_e, axis=-1)
    top_w = top_w / jnp.sum(top_w, axis=-1, keepdims=True) * ROUTED_SCALE
    a = t * TOP_K
    e_flat = top_e.reshape(a)
    w_flat = top_w.reshape(a).astype(hf.dtype)
    tok_flat = jnp.repeat(jnp.arange(t, dtype=jnp.int32), TOP_K)
    order = jnp.argsort(e_flat)
    e_sorted = e_flat[order]
    counts = jnp.zeros((N_EXPERTS,), jnp.int32).at[e_flat].add(1)
    padded = (counts + EXPERT_BLOCK - 1) // EXPERT_BLOCK * EXPERT_BLOCK
    pad_end = jnp.cumsum(padded)
    pad_start = pad_end - padded
    start = jnp.cumsum(counts) - counts
    dest = pad_start[e_sorted] + jnp.arange(a, dtype=jnp.int32) - start[e_sorted]
    n_blocks = (a + N_EXPERTS * (EXPERT_BLOCK - 1) + EXPERT_BLOCK - 1) // EXPERT_BLOCK
    p = n_blocks * EXPERT_BLOCK
    buf_tok = jnp.full((p,), t, jnp.int32).at[dest].set(tok_flat[order])
    buf_w = jnp.zeros((p,), hf.dtype).at[dest].set(w_flat[order])
    block_expert = jnp.minimum(
        jnp.searchsorted(pad_end, jnp.arange(n_blocks, dtype=jnp.int32) * EXPERT_BLOCK,
                         side='right'), N_EXPERTS - 1)
    hp = jnp.concatenate([hf, jnp.zeros((1, d), hf.dtype)], axis=0)

    def block_fn(args):
        tok, wb, e = args
        xb = hp[tok]
        return swiglu(xb, w_exp_gate[e], w_exp_up[e], w_exp_down[e]) * wb[:, None]

    out = lax.map(block_fn, (buf_tok.reshape(n_blocks, EXPERT_BLOCK),
                             buf_w.reshape(n_blocks, EXPERT_BLOCK), block_expert))
    routed = jax.ops.segment_sum(out.reshape(p, d), buf_tok, num_segments=t + 1)[:t]
    shared = swiglu(hf, w_sh_gate, w_sh_up, w_sh_down)
    return (routed + shared).reshape(b, s, d)


def setup_inputs(seed: int = 0) -> dict:
    key = jax.random.key(seed)
    ks = jax.random.split(key, 24)
    nrm = lambda k, shape, sc: jax.random.normal(k, shape, jnp.float32) * sc
    return {
        "x": nrm(ks[0], (BATCH, SEQ, D_MODEL), 1.0),
        "c": nrm(ks[1], (BATCH, D_MODEL), 1.0),
        "w_ada": nrm(ks[2], (DEPTH, D_MODEL, 6 * D_MODEL), 0.1 * D_MODEL ** -0.5),
        "b_ada": nrm(ks[3], (DEPTH, 6 * D_MODEL), 0.1),
        "norm1_g": 1.0 + nrm(ks[4], (DEPTH, D_MODEL), 0.02),
        "w_in": nrm(ks[5], (DEPTH, D_MODEL, IN_WIDTH), D_MODEL ** -0.5),
        "b_forget": 3.0 + nrm(ks[6], (DEPTH, FOX_HEADS), 0.5),
        "ret_gn_g": 1.0 + nrm(ks[7], (DEPTH, RET_V_WIDTH), 0.02),
        "w_branch_a": nrm(ks[8], (DEPTH, FOX_WIDTH, D_MODEL), FOX_WIDTH ** -0.5),
        "w_branch_b": nrm(ks[9], (DEPTH, RET_V_WIDTH, D_MODEL), RET_V_WIDTH ** -0.5),
        "w_out": nrm(ks[10], (DEPTH, D_MODEL, D_MODEL), D_MODEL ** -0.5),
        "norm2_g": 1.0 + nrm(ks[11], (DEPTH, D_MODEL), 0.02),
        "w_router": nrm(ks[12], (DEPTH, D_MODEL, N_EXPERTS), D_MODEL ** -0.5),
        "router_bias": nrm(ks[13], (DEPTH, N_EXPERTS), 0.01),
        "w_exp_gate": nrm(ks[14], (DEPTH, N_EXPERTS, D_MODEL, EXPERT_DIM), D_MODEL ** -0.5),
        "w_exp_up": nrm(ks[15], (DEPTH, N_EXPERTS, D_MODEL, EXPERT_DIM), D_MODEL ** -0.5),
        "w_exp_down": nrm(ks[16], (DEPTH, N_EXPERTS, EXPERT_DIM, D_MODEL), EXPERT_DIM ** -0.5),
        "w_sh_gate": nrm(ks[17], (DEPTH, D_MODEL, SHARED_DIM), D_MODEL ** -0.5),
        "w_sh_up": nrm(ks[18], (DEPTH, D_MODEL, SHARED_DIM), D_MODEL ** -0.5),
        "w_sh_down": nrm(ks[19], (DEPTH, SHARED_DIM, D_MODEL), SHARED_DIM ** -0.5),
        "final_g": 1.0 + nrm(ks[20], (D_MODEL,), 0.02),
    }


def reference(x, c, w_ada, b_ada, norm1_g, w_in, b_forget, ret_gn_g, w_branch_a,
              w_branch_b, w_out, norm2_g, w_router, router_bias, w_exp_gate, w_exp_up,
              w_exp_down, w_sh_gate, w_sh_up, w_sh_down, final_g):
    for l in range(DEPTH):
        mod = jax.nn.silu(c) @ w_ada[l] + b_ada[l]
        shift1, scale1, gate1, shift2, scale2, gate2 = jnp.split(mod[:, None, :], 6, axis=-1)
        h = rms_norm(x, norm1_g[l]) * (1.0 + scale1) + shift1
        x = x + gate1 * mixing_sublayer(h, w_in[l], b_forget[l], ret_gn_g[l],
                                        w_branch_a[l], w_branch_b[l], w_out[l])
        h = rms_norm(x, norm2_g[l]) * (1.0 + scale2) + shift2
        x = x + gate2 * moe_sublayer(h, w_router[l], router_bias[l], w_exp_gate[l],
                                     w_exp_up[l], w_exp_down[l], w_sh_gate[l],
                                     w_sh_up[l], w_sh_down[l])
    return rms_norm(x, final_g)
```

```python
from concourse.bass_utils import run_bass_kernel_spmd
import concourse.bass as bass
import concourse.mybir as mybir

F32 = mybir.dt.float32
BF16 = mybir.dt.bfloat16
I32 = mybir.dt.int32
U32 = mybir.dt.uint32
U8 = mybir.dt.uint8
ALU = mybir.AluOpType
AF = mybir.ActivationFunctionType
AX = mybir.AxisListType

PE, ACT, DVE, POOL, SP = "pe", "act", "dve", "pool", "sp"
COMPUTE = (PE, ACT, DVE, POOL)


class Buf:
    _n = 0

    def __init__(self, t, nparts=1, name=None):
        self.t = t
        self.nparts = nparts
        Buf._n += 1
        self.id = Buf._n
        self.name = name

    def keys(self, parts=None):
        if parts is None:
            return [(self.id, p) for p in range(self.nparts)]
        if isinstance(parts, int):
            return [(self.id, parts)]
        return [(self.id, p) for p in parts]

    def __getitem__(self, idx):
        return self.t[idx]


class Acc:
    def __init__(self, buf, parts=None):
        self.buf = buf
        self.parts = parts

    def keys(self):
        return self.buf.keys(self.parts)


def _keys(lst):
    out = []
    for a in lst:
        if a is None:
            continue
        if isinstance(a, Buf):
            out.extend(a.keys())
        elif isinstance(a, Acc):
            out.extend(a.keys())
        elif isinstance(a, tuple) and isinstance(a[0], Buf):
            out.extend(a[0].keys(a[1]))
        else:
            raise TypeError(a)
    return out


class Op:
    __slots__ = ("eng", "fn", "rk", "wk", "dma", "idx", "eidx", "waits", "signal",
                 "sem", "semval", "barrier", "deps", "sigidx")

    def __init__(self, eng, fn, rk, wk, dma):
        self.eng = eng
        self.fn = fn
        self.rk = rk
        self.wk = wk
        self.dma = dma
        self.waits = []
        self.signal = False
        self.sem = None
        self.semval = None
        self.barrier = False
        self.deps = None


import types


def _freeze(fn):
    if fn.__closure__ is None:
        return fn
    cells = []
    for c in fn.__closure__:
        try:
            cells.append(types.CellType(c.cell_contents))
        except ValueError:
            cells.append(c)
    g = types.FunctionType(fn.__code__, fn.__globals__, fn.__name__, fn.__defaults__, tuple(cells))
    g.__kwdefaults__ = fn.__kwdefaults__
    return g


class Sched:
    SEM_EPOCH = 20000
    N_DMA_SEMS_SP = 24
    N_DMA_SEMS_POOL = 40

    def __init__(self, nc):
        self.nc = nc
        self.ops = []

    def op(self, eng, fn, reads=(), writes=()):
        o = Op(eng, _freeze(fn), _keys(reads), _keys(writes), False)
        o.idx = len(self.ops)
        self.ops.append(o)
        return o

    def dma(self, eng, fn, reads=(), writes=()):
        o = Op(eng, _freeze(fn), _keys(reads), _keys(writes), True)
        o.idx = len(self.ops)
        self.ops.append(o)
        return o

    def barrier(self):
        o = Op(None, None, [], [], False)
        o.barrier = True
        o.idx = len(self.ops)
        self.ops.append(o)

    def analyze(self):
        last_w = {}
        readers = {}
        engs = (PE, ACT, DVE, POOL, SP)
        eng_ops = {e: [] for e in engs}
        last_on_eng = {e: None for e in engs}
        dma_since_barrier = []
        pending_front = {e: [] for e in engs}
        ops = self.ops
        for o in ops:
            if o.barrier:
                front = [v for e, v in last_on_eng.items() if v is not None and e in COMPUTE]
                front += dma_since_barrier
                dma_since_barrier = []
                for e in engs:
                    pending_front[e] = list(front)
                last_w.clear()
                readers.clear()
                continue
            deps = set()
            for k in o.rk:
                w = last_w.get(k)
                if w is not None:
                    deps.add((w, "raw"))
            for k in o.wk:
                w = last_w.get(k)
                if w is not None:
                    deps.add((w, "waw"))
                for r in readers.get(k, ()):
                    deps.add((r, "war"))
            if pending_front[o.eng]:
                for f in pending_front[o.eng]:
                    deps.add((f, "bar"))
                pending_front[o.eng] = []
            o.deps = deps
            for k in o.rk:
                readers.setdefault(k, []).append(o.idx)
            for k in o.wk:
                last_w[k] = o.idx
                readers[k] = []
            o.eidx = len(eng_ops[o.eng])
            eng_ops[o.eng].append(o)
            if o.dma:
                dma_since_barrier.append(o.idx)
            else:
                last_on_eng[o.eng] = o.idx
        self.eng_ops = eng_ops
        for o in ops:
            if o.barrier:
                continue
            per_eng = {}
            dma_deps = set()
            for (d, kind) in o.deps:
                if d == o.idx:
                    continue
                p = ops[d]
                if p.dma:
                    dma_deps.add(d)
                    continue
                if p.eng == o.eng and not o.dma:
                    if o.eng == PE:
                        continue
                    if kind not in ("raw", "war", "waw"):
                        continue
                    if o.eidx - p.eidx > 3:
                        continue
                prev = per_eng.get(p.eng)
                if prev is None or p.eidx > ops[prev].eidx:
                    per_eng[p.eng] = d
            o.waits = list(per_eng.values()) + sorted(dma_deps)
            for d in o.waits:
                ops[d].signal = True
        for o in ops:
            if not o.barrier and o.dma:
                o.signal = True

    def emit(self, final_wait_engine=SP):
        nc = self.nc
        ops = self.ops
        import contextlib
        self._stack = contextlib.ExitStack()
        st = self._stack
        eng_sems = {}
        for e in COMPUTE:
            nsig = sum(1 for o in self.eng_ops[e] if o.signal and not o.dma)
            nep = nsig // self.SEM_EPOCH + 1
            eng_sems[e] = [st.enter_context(nc.semaphore(f"s_{e}_{i}")) for i in range(nep)]
            c = 0
            for o in self.eng_ops[e]:
                if o.signal and not o.dma:
                    o.sem = eng_sems[e][c // self.SEM_EPOCH]
                    o.semval = c % self.SEM_EPOCH + 1
                    o.sigidx = c
                    c += 1
        pools = {}
        for qn, nsem in ((SP, self.N_DMA_SEMS_SP), (POOL, self.N_DMA_SEMS_POOL), (ACT, 4)):
            if any(o.dma and o.eng == qn for o in ops if not o.barrier):
                pools[qn] = dict(sems=[st.enter_context(nc.semaphore(f"s_dma_{qn}_{i}")) for i in range(nsem)],
                                 counts=[0] * nsem, last=[None] * nsem, di=0)
        for o in ops:
            if o.barrier or not o.dma:
                continue
            P = pools[o.eng]
            n = len(P["sems"])
            s = P["di"] % n
            P["di"] += 1
            prev = P["last"][s]
            if prev is not None and prev not in o.waits:
                o.waits.append(prev)
            P["counts"][s] += 1
            o.sem = P["sems"][s]
            o.semval = 16 * P["counts"][s]
            P["last"][s] = o.idx
            if o.semval > 60000:
                raise RuntimeError("dma sem overflow")
        self.n_wait_insts = 0

        def run_engine(ename, eng):
            known_eng = {}
            known_dma = {}
            for o in self.eng_ops[ename]:
                for d in o.waits:
                    p = ops[d]
                    if p.dma:
                        key = id(p.sem)
                        if known_dma.get(key, 0) >= p.semval:
                            continue
                        known_dma[key] = p.semval
                        eng.wait_ge(p.sem, p.semval)
                        self.n_wait_insts += 1
                    else:
                        if known_eng.get(p.eng, -1) >= p.sigidx:
                            continue
                        known_eng[p.eng] = p.sigidx
                        eng.wait_ge(p.sem, p.semval)
                        self.n_wait_insts += 1
                ins = o.fn(eng)
                if o.signal:
                    ins.then_inc(o.sem, 16 if o.dma else 1)
            return known_eng, known_dma

        last_dma = [o for o in ops if not o.barrier and o.dma]
        with nc.Block() as block:
            @block.tensor
            def _(e):
                run_engine(PE, e)

            @block.scalar
            def _(e):
                run_engine(ACT, e)

            @block.vector
            def _(e):
                run_engine(DVE, e)

            @block.gpsimd
            def _(e):
                run_engine(POOL, e)

            @block.sync
            def _(e):
                ke, kd = run_engine(SP, e)
                finals = {}
                for o in last_dma:
                    key = id(o.sem)
                    if finals.get(key, (None, 0))[1] < o.semval:
                        finals[key] = (o.sem, o.semval)
                for sem, val in finals.values():
                    e.wait_ge(sem, val)
                for en in COMPUTE:
                    sig = [o for o in self.eng_ops[en] if o.signal and not o.dma]
                    if sig:
                        e.wait_ge(sig[-1].sem, sig[-1].semval)
        st.close()


class Arena:
    def __init__(self, nc, base, limit):
        self.nc = nc
        self.base = base
        self.top = base
        self.limit = limit
        self.n = 0

    def alloc(self, shape, dtype, name=None, nparts=1):
        esz = {F32: 4, BF16: 2, I32: 4, U32: 4, U8: 1}[dtype]
        n = 1
        for s in shape[1:]:
            n *= s
        nbytes = (n * esz + 63) // 64 * 64
        off = self.top
        if off + nbytes > self.limit:
            raise MemoryError(f"SBUF arena overflow: {name} needs {nbytes}, top={off}, limit={self.limit}")
        self.top += nbytes
        self.n += 1
        t = self.nc.alloc_sbuf_tensor_at(f"{name or 't'}_{self.n}", list(shape), dtype, offset=off)
        return Buf(t, nparts, name)

    def mark(self):
        return self.top

    def release(self, m):
        self.top = m


import numpy as np

NT = 16
NE = 256
CAPW = 257


def build_program():
    nc = bass.Bass("TRN2", target_bir_lowering=False)

    def din(name, shape, dt=F32):
        return nc.dram_tensor(name, list(shape), dt, kind="ExternalInput")

    xo = din("xo", [2048, 1024]); xc = din("xc", [2048, 1024])
    cin = din("cin", [128, 8]); flag_d = din("flag", [128, 1])
    w_ada = din("w_ada", [1024, 6144]); b_ada_bc = din("b_ada_bc", [128, 6144])
    g1_bc = din("g1_bc", [128, 1024]); g2_bc = din("g2_bc", [128, 1024]); fg_bc = din("fg_bc", [128, 1024])
    w_in = din("w_in", [1024, 6664]); bfg_bc = din("bfg_bc", [128, 8]); gng_d = din("gng", [128, 8])
    w_ba = din("w_ba", [512, 1024]); w_bb = din("w_bb", [1024, 1024]); w_out = din("w_out", [1024, 1024])
    w_router = din("w_router", [1024, 256]); rbias_bc = din("rbias_bc", [128, 256])
    w_eg = din("w_eg", [256, 1024, 256]); w_eu = din("w_eu", [256, 1024, 256]); w_ed = din("w_ed", [256, 256, 1024])
    w_sg = din("w_sg", [1024, 256]); w_su = din("w_su", [1024, 256]); w_sd = din("w_sd", [256, 1024])
    ident_d = din("ident", [128, 128]); tri_d = din("tri", [128, 128])
    rot = {k: din(k, [2048, 256]) for k in ("rkc_c", "rkc_s", "rko_c", "rko_s", "rqo_c", "rqo_s")}
    cdt_d = din("cdt", [128, 512])
    iota_d = din("iota", [128, 16], I32)
    out_d = nc.dram_tensor("out", [2048, 1024], F32, kind="ExternalOutput")
    YA = nc.dram_tensor("ya_scr", [16, 128, 1024], BF16)
    X1 = nc.dram_tensor("x1_scr", [2048, 1024], F32)
    YAb = Buf(YA, 16); X1b = Buf(X1, 16)

    ARENA_BYTES = 210000
    arena_t = nc.alloc_sbuf_tensor("arena", [128, ARENA_BYTES], U8)
    base = nc.lookup_mloc(arena_t).addr
    A = Arena(nc, base, base + ARENA_BYTES)
    S = Sched(nc)
    PS = [Buf(nc.alloc_psum_tensor(f"psb{i}", [128, 512], F32), 4, f"ps{i}") for i in range(8)]
    PSB = [p.t.bitcast(BF16) for p in PS]

    def cols(ap2d, a):
        return ap2d.rearrange("p (a c) -> p a c", a=a)

    identf = A.alloc([128, 128], F32, "identf"); identb = A.alloc([128, 128], BF16, "identb")
    trif = A.alloc([128, 128], F32, "trif"); trib = A.alloc([128, 128], BF16, "trib")
    onesf = A.alloc([128, 128], F32, "onesf")
    flag = A.alloc([128, 1], F32, "flag"); omf = A.alloc([128, 1], F32, "omf")
    mod = A.alloc([128, 6, 1024], F32, "mod", 6)
    fgt = A.alloc([128, 1024], F32, "fgt")
    S.dma(SP, lambda e: e.dma_start(out=identf[:, :], in_=ident_d[:, :]), [], [identf])
    S.dma(SP, lambda e: e.dma_start(out=trif[:, :], in_=tri_d[:, :]), [], [trif])
    S.dma(SP, lambda e: e.dma_start(out=flag[:, :], in_=flag_d[:, :]), [], [flag])
    S.dma(SP, lambda e: e.dma_start(out=fgt[:, :], in_=fg_bc[:, :]), [], [fgt])
    S.op(DVE, lambda e: e.tensor_copy(out=identb[:, :], in_=identf[:, :]), [identf], [identb])
    S.op(DVE, lambda e: e.tensor_copy(out=trib[:, :], in_=trif[:, :]), [trif], [trib])
    S.op(DVE, lambda e: e.memset(onesf[:, :], 1.0), [], [onesf])
    S.op(DVE, lambda e: e.tensor_scalar(out=omf[:, :], in0=flag[:, :], scalar1=-1.0, scalar2=30000.0,
                                        op0=ALU.add, op1=ALU.mult), [flag], [omf])

    mA = A.mark()
    cs = A.alloc([128, 8], F32, "cs"); csg = A.alloc([128, 8], F32, "csg")
    scb = A.alloc([128, 8, 128], F32, "scb")
    wada = [A.alloc([128, 8, 512], F32, f"wada{i}") for i in range(2)]
    btmp = [A.alloc([128, 512], F32, f"btmp{i}") for i in range(2)]
    gtmp = A.alloc([128, 1024], F32, "gtmp")
    S.dma(SP, lambda e: e.dma_start(out=cs[:, :], in_=cin[:, :]), [], [cs])
    S.op(ACT, lambda e: e.activation(out=csg[:, :], in_=cs[:, :], func=AF.Sigmoid), [cs], [csg])
    S.op(DVE, lambda e: e.tensor_tensor(out=csg[:, :], in0=csg[:, :], in1=cs[:, :], op=ALU.mult), [csg, cs], [csg])
    for k in range(8):
        S.op(DVE, lambda e, k=k: e.tensor_scalar(out=scb[:, k, :], in0=onesf[:, :], scalar1=csg[:, k:k + 1], scalar2=None,
                                                 op0=ALU.mult), [onesf, csg], [scb])
    wada_v = w_ada.ap().rearrange("(k p) c -> p k c", p=128)
    for j in range(12):
        wt = wada[j % 2]; bt = btmp[j % 2]; ps = PS[j % 2]
        S.dma(SP, lambda e, wt=wt, j=j: e.dma_start(out=wt[:, :, :], in_=wada_v[:, :, j * 512:(j + 1) * 512]), [], [wt])
        S.dma(SP, lambda e, bt=bt, j=j: e.dma_start(out=bt[:, :], in_=b_ada_bc[:, j * 512:(j + 1) * 512]), [], [bt])
        for k in range(8):
            S.op(PE, lambda e, ps=ps, wt=wt, k=k: e.matmul(ps[:, 0:512], lhsT=scb[:, k, :], rhs=wt[:, k, :],
                                                          start=(k == 0), stop=(k == 7)), [scb, wt], [ps])
        S.op(DVE, lambda e, ps=ps, bt=bt, j=j: e.tensor_tensor(out=mod[:, j // 2, (j % 2) * 512:(j % 2 + 1) * 512],
                                                               in0=ps[:, 0:512], in1=bt[:, :], op=ALU.add),
             [ps, bt], [(mod, j // 2)])
    for (slot, gsrc) in ((1, g1_bc), (4, g2_bc)):
        S.dma(SP, lambda e, gsrc=gsrc: e.dma_start(out=gtmp[:, :], in_=gsrc[:, :]), [], [gtmp])
        S.op(DVE, lambda e, slot=slot: e.scalar_tensor_tensor(out=mod[:, slot, :], in0=mod[:, slot, :], scalar=1.0,
                                                              in1=gtmp[:, :], op0=ALU.add, op1=ALU.mult),
             [(mod, slot), gtmp], [(mod, slot)])
    A.release(mA)
    S.barrier()

    def load_w(dst, src_ap):
        S.dma(POOL, lambda e: e.dma_start(out=dst[:, :, :], in_=src_ap), [], [dst])

    def win_cols(c0, c1):
        return w_in.ap()[:, c0:c1].rearrange("(k p) c -> p k c", p=128)

    class NormCtx:
        def __init__(self):
            self.x = [A.alloc([128, 1024], F32, f"nx{i}") for i in range(2)]
            self.junk = A.alloc([128, 1024], BF16, "njunk")
            self.t = A.alloc([128, 1024], F32, "nt")
            self.h = [A.alloc([128, 1024], BF16, f"nh{i}") for i in range(2)]
            self.hT = [A.alloc([128, 8, 128], BF16, f"nhT{i}") for i in range(2)]
            self.ss = [A.alloc([128, 1], F32, f"nss{i}") for i in range(2)]
            self.rs = [A.alloc([128, 1], F32, f"nrs{i}") for i in range(2)]
            self.n = 0

    def rstd_of(ss, rs, x_ap, xbuf, junk):
        S.op(DVE, lambda e: e.memset(ss[:, :], 0.0), [], [ss])
        S.op(ACT, lambda e: e.activation(out=junk[:, :], in_=x_ap, func=AF.Square, accum_out=ss[:, 0:1]), [xbuf], [junk, ss])
        S.op(DVE, lambda e: e.tensor_scalar(out=rs[:, :], in0=ss[:, :], scalar1=1.0 / 1024, scalar2=1e-6,
                                            op0=ALU.mult, op1=ALU.add), [ss], [rs])
        S.op(ACT, lambda e: e.activation(out=rs[:, :], in_=rs[:, :], func=AF.Ln), [rs], [rs])
        S.op(ACT, lambda e: e.activation(out=rs[:, :], in_=rs[:, :], func=AF.Exp, scale=-0.5), [rs], [rs])

    def norm_hT(ctx, src_rows_ap, src_dep, a_slot, b_slot, psT, want_h=False):
        i = ctx.n % 2; ctx.n += 1
        x = ctx.x[i]; h = ctx.h[i]; hT = ctx.hT[i]; ss = ctx.ss[i]; rs = ctx.rs[i]
        S.dma(SP, lambda e: e.dma_start(out=x[:, :], in_=src_rows_ap), src_dep, [x])
        rstd_of(ss, rs, x[:, :], x, ctx.junk)
        S.op(DVE, lambda e: e.scalar_tensor_tensor(out=ctx.t[:, :], in0=x[:, :], scalar=rs[:, 0:1], in1=mod[:, a_slot, :],
                                                   op0=ALU.mult, op1=ALU.mult), [x, rs, (mod, a_slot)], [ctx.t])
        S.op(DVE, lambda e: e.tensor_tensor(out=h[:, :], in0=ctx.t[:, :], in1=mod[:, b_slot, :], op=ALU.add),
             [ctx.t, (mod, b_slot)], [h])
        pb = PSB[psT]
        for k in range(8):
            S.op(PE, lambda e, k=k: e.transpose(out=pb[:, k * 128:(k + 1) * 128], in_=h[:, k * 128:(k + 1) * 128],
                                                identity=identb[:, :]), [h, identb], [PS[psT]])
        S.op(ACT, lambda e: e.copy(out=hT[:, :, :], in_=cols(pb[:, 0:1024], 8)), [PS[psT]], [hT])
        return (hT, h, x) if want_h else hT

    xo_v = xo.ap().rearrange("(t p) c -> t p c", p=128)
    xc_v = xc.ap().rearrange("(t p) c -> t p c", p=128)

    m1 = A.mark()
    wq = A.alloc([128, 8, 512], BF16, "wq"); wk = A.alloc([128, 8, 512], BF16, "wk")
    wv = A.alloc([128, 8, 512], BF16, "wv"); wf = A.alloc([128, 8, 8], BF16, "wf")
    wba = A.alloc([128, 4, 1024], BF16, "wba")
    load_w(wk, win_cols(512, 1024)); load_w(wv, win_cols(1024, 1536)); load_w(wf, win_cols(1536, 1544))
    load_w(wq, win_cols(0, 512))
    load_w(wba, w_ba.ap().rearrange("(k p) c -> p k c", p=128))
    kT_all = A.alloc([128, 4, 4096], BF16, "kT_all", 32)
    V_all = A.alloc([128, 32, 8, 65], BF16, "V_all", 32)
    G_all = A.alloc([128, 32, 8], F32, "G_all", 32)
    Gend = A.alloc([128, 33, 8], F32, "Gend", 33)
    bfg = A.alloc([128, 8], F32, "bfg")
    S.dma(SP, lambda e: e.dma_start(out=bfg[:, :], in_=bfg_bc[:, :]), [], [bfg])
    S.op(DVE, lambda e: e.memset(V_all[:, :, :, 64:65], 1.0), [], [V_all])
    S.op(DVE, lambda e: e.memset(Gend[:, 0, :], 0.0), [], [(Gend, 0)])
    ctx = NormCtx()
    lf = [A.alloc([128, 8], F32, f"lf{i}") for i in range(2)]
    qT = [A.alloc([128, 4, 128], BF16, f"qT{i}") for i in range(2)]
    biasb = [A.alloc([128, 8, 32], F32, f"biasb{i}") for i in range(2)]
    PT = [A.alloc([128, 128], BF16, f"PT{i}") for i in range(8)]
    rden = [A.alloc([128, 8], F32, f"rden{i}") for i in range(2)]
    ao = [A.alloc([128, 512], BF16, f"ao{i}") for i in range(2)]
    aoT = [A.alloc([128, 4, 128], BF16, f"aoT{i}") for i in range(2)]
    yab = [A.alloc([128, 8, 128], BF16, f"yab{i}") for i in range(2)]
    ptc = [0]

    def fox_kvf(hT, gb, it):
        for pr in range(4):
            for k in range(8):
                S.op(PE, lambda e, pr=pr, k=k: e.matmul(PS[1][:, pr * 128:(pr + 1) * 128], lhsT=wk[:, k, pr * 128:(pr + 1) * 128],
                                                        rhs=hT[:, k, :], start=(k == 0), stop=(k == 7)), [wk, hT], [PS[1]])
        S.op(ACT, lambda e: e.copy(out=kT_all[:, :, gb * 128:(gb + 1) * 128], in_=cols(PS[1][:, 0:512], 4)),
             [PS[1]], [(kT_all, gb)])
        for k in range(8):
            S.op(PE, lambda e, k=k: e.matmul(PS[2][:, 0:512], lhsT=hT[:, k, :], rhs=wv[:, k, :],
                                             start=(k == 0), stop=(k == 7)), [wv, hT], [PS[2]])
        S.op(DVE, lambda e: e.tensor_copy(out=V_all[:, gb, :, 0:64], in_=cols(PS[2][:, 0:512], 8)), [PS[2]], [(V_all, gb)])
        for k in range(8):
            S.op(PE, lambda e, k=k: e.matmul(PS[4][:, 384:392], lhsT=hT[:, k, :], rhs=wf[:, k, :],
                                             start=(k == 0), stop=(k == 7)), [wf, hT], [(PS[4], 3)])
        l = lf[it % 2]
        S.op(DVE, lambda e: e.tensor_tensor(out=l[:, :], in0=PS[4][:, 384:392], in1=bfg[:, :], op=ALU.add), [(PS[4], 3), bfg], [l])
        S.op(ACT, lambda e: e.activation(out=l[:, :], in_=l[:, :], func=AF.Exp, scale=-1.0), [l], [l])
        S.op(ACT, lambda e: e.activation(out=l[:, :], in_=l[:, :], func=AF.Ln, bias=1.0), [l], [l])
        S.op(PE, lambda e: e.matmul(PS[4][:, 400:408], lhsT=trif[:, :], rhs=l[:, :], start=True, stop=True), [trif, l], [(PS[4], 3)])
        S.op(PE, lambda e: e.matmul(PS[4][:, 416:424], lhsT=onesf[:, :], rhs=l[:, :], start=True, stop=True), [onesf, l], [(PS[4], 3)])
        S.op(DVE, lambda e: e.tensor_tensor(out=G_all[:, gb, :], in0=PS[4][:, 400:408], in1=Gend[:, gb, :], op=ALU.add),
             [(PS[4], 3), (Gend, gb)], [(G_all, gb)])
        S.op(DVE, lambda e: e.tensor_tensor(out=Gend[:, gb + 1, :], in0=PS[4][:, 416:424], in1=Gend[:, gb, :], op=ALU.add),
             [(PS[4], 3), (Gend, gb)], [(Gend, gb + 1)])

    for it in range(NT):
        hT = norm_hT(ctx, xc_v[it], [], 1, 0, 0)
        fox_kvf(hT, it, it)
    S.op(DVE, lambda e: e.tensor_scalar(out=G_all[:, 0:16, :], in0=G_all[:, 0:16, :], scalar1=omf[:, 0:1], scalar2=None,
                                        op0=ALU.add), [(G_all, range(16)), omf], [(G_all, range(16))])
    S.op(DVE, lambda e: e.tensor_scalar(out=Gend[:, 16, :], in0=Gend[:, 16, :], scalar1=flag[:, 0:1], scalar2=None,
                                        op0=ALU.mult), [(Gend, 16), flag], [(Gend, 16)])

    for it in range(NT):
        gb = 16 + it
        hT = norm_hT(ctx, xo_v[it], [], 1, 0, 0)
        q = qT[it % 2]
        for pr in range(4):
            for k in range(8):
                S.op(PE, lambda e, pr=pr, k=k: e.matmul(PS[3][:, pr * 128:(pr + 1) * 128], lhsT=wq[:, k, pr * 128:(pr + 1) * 128],
                                                        rhs=hT[:, k, :], start=(k == 0), stop=(k == 7)), [wq, hT], [PS[3]])
        S.op(ACT, lambda e: e.copy(out=q[:, :, :], in_=cols(PS[3][:, 0:512], 4)), [PS[3]], [q])
        fox_kvf(hT, gb, it)
        bb = biasb[it % 2]
        nk = gb + 1
        for h in range(8):
            S.op(DVE, lambda e, h=h: e.tensor_scalar(out=bb[:, h, 0:nk], in0=G_all[:, 0:nk, h], scalar1=Gend[:, gb + 1, h:h + 1],
                                                     scalar2=None, op0=ALU.subtract),
                 [(G_all, range(nk)), (Gend, gb + 1)], [bb])
        items = [(h, kb) for h in range(8) for kb in range(nk)]
        chunks = []
        for h_ in range(8):
            hi = [(h_, kb_) for kb_ in range(nk)]
            chunks += [hi[i:i + 4] for i in range(0, nk, 4)]

        def emit_qk(ci):
            sb = PS[6 + ci % 2]
            for j, (h, kb) in enumerate(chunks[ci]):
                pr, off = h // 2, (h % 2) * 64
                S.op(PE, lambda e: e.matmul(sb[:, j * 128:(j + 1) * 128], lhsT=kT_all[off:off + 64, pr, kb * 128:(kb + 1) * 128],
                                            rhs=q[off:off + 64, pr, :], start=True, stop=True), [(kT_all, kb), q], [sb])
            for j, (h, kb) in enumerate(chunks[ci]):
                pt = PT[(ci % 2) * 4 + j]
                S.op(ACT, lambda e: e.activation(out=pt[:, :], in_=sb[:, j * 128:(j + 1) * 128], func=AF.Exp,
                                                 bias=bb[:, h, kb:kb + 1], scale=0.125), [sb, bb], [pt])
                if kb == gb:
                    S.op(DVE, lambda e: e.tensor_tensor(out=pt[:, :], in0=pt[:, :], in1=trib[:, :], op=ALU.mult), [pt, trib], [pt])

        def emit_pv(ci):
            for j, (h, kb) in enumerate(chunks[ci]):
                pt = PT[(ci % 2) * 4 + j]
                ob = PS[4] if h < 4 else PS[5]
                oc = (h % 4) * 65
                S.op(PE, lambda e: e.matmul(ob[:, oc:oc + 65], lhsT=pt[:, :], rhs=V_all[:, kb, h, :], start=(kb == 0), stop=(kb == gb)),
                     [pt, (V_all, kb)], [(ob, (0, 1, 2))])

        for ci in range(len(chunks)):
            emit_qk(ci)
            if ci > 0:
                emit_pv(ci - 1)
        emit_pv(len(chunks) - 1)
        rd = rden[it % 2]; a_ = ao[it % 2]; aT = aoT[it % 2]; yb = yab[it % 2]
        for hh in range(2):
            ob = PS[4 + hh]
            ov = ob[:, 0:260].rearrange("p (h c) -> p h c", h=4)
            S.op(DVE, lambda e, ov=ov, hh=hh: e.reciprocal(out=rd[:, hh * 4:(hh + 1) * 4], in_=ov[:, :, 64]), [(ob, (0, 1, 2))], [rd])
            for h4 in range(4):
                h = hh * 4 + h4
                S.op(DVE, lambda e, ov=ov, h=h, h4=h4: e.tensor_scalar(out=a_[:, h * 64:(h + 1) * 64], in0=ov[:, h4, 0:64],
                                                                      scalar1=rd[:, h:h + 1], scalar2=None, op0=ALU.mult),
                     [(ob, (0, 1, 2)), rd], [a_])
        for j in range(4):
            S.op(PE, lambda e, j=j: e.transpose(out=PSB[0][:, j * 128:(j + 1) * 128], in_=a_[:, j * 128:(j + 1) * 128],
                                                identity=identb[:, :]), [a_, identb], [PS[0]])
        S.op(ACT, lambda e: e.copy(out=aT[:, :, :], in_=cols(PSB[0][:, 0:512], 4)), [PS[0]], [aT])
        for c in range(8):
            pbk = PS[1] if c < 4 else PS[2]
            for j in range(4):
                S.op(PE, lambda e, c=c, j=j, pbk=pbk: e.matmul(pbk[:, (c % 4) * 128:(c % 4 + 1) * 128],
                                                              lhsT=wba[:, j, c * 128:(c + 1) * 128], rhs=aT[:, j, :],
                                                              start=(j == 0), stop=(j == 3)), [wba, aT], [pbk])
        S.op(ACT, lambda e: e.copy(out=yb[:, 0:4, :], in_=cols(PS[1][:, 0:512], 4)), [PS[1]], [yb])
        S.op(ACT, lambda e: e.copy(out=yb[:, 4:8, :], in_=cols(PS[2][:, 0:512], 4)), [PS[2]], [yb])
        S.dma(SP, lambda e, it=it: e.dma_start(out=YA[it].rearrange("p (c t) -> p c t", c=8), in_=yb[:, :, :]), [yb], [(YAb, it)])
    A.release(m1)
    S.barrier()

    YB = nc.dram_tensor("yb_scr", [16, 128, 1024], BF16)
    YBb = Buf(YB, 16)
    m2 = A.mark()
    wrq = A.alloc([128, 8, 512], BF16, "wrq"); wrk = A.alloc([128, 8, 512], BF16, "wrk")
    wrv = A.alloc([128, 8, 1024], BF16, "wrv"); wrg = A.alloc([128, 8, 1024], BF16, "wrg")
    wgb = A.alloc([128, 8, 1024], BF16, "wgb"); wbb = A.alloc([128, 8, 1024], BF16, "wbb")
    load_w(wrk, win_cols(2056, 2568)); load_w(wrv, win_cols(2568, 3592)); load_w(wrq, win_cols(1544, 2056))
    load_w(wrg, win_cols(3592, 4616)); load_w(wgb, win_cols(5640, 6664))
    load_w(wbb, w_bb.ap().rearrange("(k p) c -> p k c", p=128))
    gng = A.alloc([128, 8], F32, "gng"); cdt = A.alloc([128, 4, 128], F32, "cdt")
    S.dma(SP, lambda e: e.dma_start(out=gng[:, :], in_=gng_d[:, :]), [], [gng])
    S.dma(SP, lambda e: e.dma_start(out=cdt[:, :, :], in_=cdt_d.ap().rearrange("p (m c) -> p m c", m=4)), [], [cdt])
    for h in range(8):
        S.op(DVE, lambda e, h=h: e.tensor_scalar(out=wbb[:, h, :], in0=wbb[:, h, :], scalar1=gng[:, h:h + 1], scalar2=None,
                                                 op0=ALU.mult), [wbb, gng], [wbb])
    St = A.alloc([128, 4, 128], F32, "St"); Sbf = A.alloc([128, 4, 128], BF16, "Sbf")
    S.op(DVE, lambda e: e.memset(St[:, :, :], 0.0), [], [St])
    S.op(DVE, lambda e: e.memset(Sbf[:, :, :], 0.0), [], [Sbf])
    ctx = NormCtx()
    tabs = [A.alloc([128, 256], F32, f"tab{i}") for i in range(4)]
    rt = [A.alloc([128, 8, 32], F32, f"rt{i}") for i in range(2)]
    qt = A.alloc([128, 8, 64], BF16, "qt"); kt = A.alloc([128, 8, 64], BF16, "kt")
    qtT = A.alloc([128, 4, 128], BF16, "qtT"); ktT = A.alloc([128, 4, 128], BF16, "ktT")
    vb = A.alloc([128, 8, 128], BF16, "vb")
    sig = A.alloc([128, 4, 128], F32, "sig")
    srg = A.alloc([128, 8, 128], BF16, "srg"); sgb = A.alloc([128, 8, 128], BF16, "sgb")
    msc = [A.alloc([128, 128], BF16, f"msc{i}") for i in range(4)]
    osb = A.alloc([128, 8, 128], F32, "osb"); osq = A.alloc([128, 8, 128], F32, "osq")
    mean = A.alloc([128, 8, 128], F32, "mean"); var = A.alloc([128, 8, 128], F32, "var")
    og = A.alloc([128, 8, 128], BF16, "og"); ybg = [A.alloc([128, 8, 128], BF16, f"ybg{i}") for i in range(2)]
    onesn = A.alloc([128, 128], F32, "onesn")
    S.op(DVE, lambda e: e.memset(onesn[:, :], 1.0 / 128), [], [onesn])

    def rotary(psb, tc, ts, dst):
        xv = cols(psb[:, 0:512], 8)
        x1, x2 = xv[:, :, 0:32], xv[:, :, 32:64]
        C = cols(tc[:, :], 8); Sn = cols(ts[:, :], 8)
        S.op(DVE, lambda e: e.tensor_tensor(out=rt[0][:, :, :], in0=x1, in1=C, op=ALU.mult), [psb, tc], [rt[0]])
        S.op(DVE, lambda e: e.tensor_tensor(out=rt[1][:, :, :], in0=x2, in1=Sn, op=ALU.mult), [psb, ts], [rt[1]])
        S.op(DVE, lambda e: e.tensor_tensor(out=dst[:, :, 0:32], in0=rt[0][:, :, :], in1=rt[1][:, :, :], op=ALU.subtract),
             [rt[0], rt[1]], [dst])
        S.op(DVE, lambda e: e.tensor_tensor(out=rt[0][:, :, :], in0=x1, in1=Sn, op=ALU.mult), [psb, ts], [rt[0]])
        S.op(DVE, lambda e: e.tensor_tensor(out=rt[1][:, :, :], in0=x2, in1=C, op=ALU.mult), [psb, tc], [rt[1]])
        S.op(DVE, lambda e: e.tensor_tensor(out=dst[:, :, 32:64], in0=rt[0][:, :, :], in1=rt[1][:, :, :], op=ALU.add),
             [rt[0], rt[1]], [dst])

    def ret_kv(hT, it, kc_name, ks_name):
        S.dma(SP, lambda e: e.dma_start(out=tabs[0][:, :], in_=rot[kc_name][it * 128:(it + 1) * 128, :]), [], [tabs[0]])
        S.dma(SP, lambda e: e.dma_start(out=tabs[1][:, :], in_=rot[ks_name][it * 128:(it + 1) * 128, :]), [], [tabs[1]])
        for k in range(8):
            S.op(PE, lambda e, k=k: e.matmul(PS[2][:, 0:512], lhsT=hT[:, k, :], rhs=wrk[:, k, :], start=(k == 0), stop=(k == 7)),
                 [hT, wrk], [PS[2]])
        for hh in range(2):
            for k in range(8):
                S.op(PE, lambda e, k=k, hh=hh: e.matmul(PS[3 + hh][:, 0:512], lhsT=hT[:, k, :], rhs=wrv[:, k, hh * 512:(hh + 1) * 512],
                                                        start=(k == 0), stop=(k == 7)), [hT, wrv], [PS[3 + hh]])
        rotary(PS[2], tabs[0], tabs[1], kt)
        for hh in range(2):
            S.op(ACT, lambda e, hh=hh: e.copy(out=vb[:, hh * 4:(hh + 1) * 4, :], in_=cols(PS[3 + hh][:, 0:512], 4)), [PS[3 + hh]], [vb])

    def state_update():
        for h in range(8):
            pr = h // 2
            pb_ = PS[6 + h // 4]
            S.op(PE, lambda e, h=h, pr=pr, pb_=pb_: e.matmul(pb_[:, (h % 4) * 128:(h % 4 + 1) * 128],
                                                            lhsT=kt[:, :, :].rearrange("p h c -> p (h c)")[:, pr * 128:(pr + 1) * 128],
                                                            rhs=vb[:, h, :], start=True, stop=True), [kt, vb], [pb_])
        for bi in range(2):
            for par in range(2):
                src = PS[6 + bi][par * 64:(par + 1) * 64, 0:512].rearrange("p (m two c) -> p m two c", two=2, c=128)[:, :, par, :]
                dst = St[par * 64:(par + 1) * 64, 2 * bi:2 * bi + 2, :]
                S.op(DVE, lambda e, src=src, dst=dst: e.tensor_tensor(out=dst, in0=src, in1=dst, op=ALU.add), [PS[6 + bi], St], [St])
        S.op(DVE, lambda e: e.tensor_tensor(out=St[:, :, :], in0=St[:, :, :], in1=cdt[:, :, :], op=ALU.mult), [St, cdt], [St])

    for it in range(NT):
        hT = norm_hT(ctx, xc_v[it], [], 1, 0, 0)
        ret_kv(hT, it, "rkc_c", "rkc_s")
        state_update()
    S.op(DVE, lambda e: e.tensor_scalar(out=St[:, :, :], in0=St[:, :, :], scalar1=flag[:, 0:1], scalar2=None, op0=ALU.mult),
         [St, flag], [St])
    S.op(DVE, lambda e: e.tensor_copy(out=Sbf[:, :, :], in_=St[:, :, :]), [St], [Sbf])

    for it in range(NT):
        hT = norm_hT(ctx, xo_v[it], [], 1, 0, 0)
        S.dma(SP, lambda e, it=it: e.dma_start(out=tabs[2][:, :], in_=rot["rqo_c"][it * 128:(it + 1) * 128, :]), [], [tabs[2]])
        S.dma(SP, lambda e, it=it: e.dma_start(out=tabs[3][:, :], in_=rot["rqo_s"][it * 128:(it + 1) * 128, :]), [], [tabs[3]])
        for k in range(8):
            S.op(PE, lambda e, k=k: e.matmul(PS[1][:, 0:512], lhsT=hT[:, k, :], rhs=wrq[:, k, :], start=(k == 0), stop=(k == 7)),
                 [hT, wrq], [PS[1]])
        ret_kv(hT, it, "rko_c", "rko_s")
        rotary(PS[1], tabs[2], tabs[3], qt)
        for (wsrc, dst, is_silu) in ((wrg, srg, True), (wgb, sgb, False)):
            for hh in range(2):
                for c4 in range(4):
                    c = hh * 4 + c4
                    for k in range(8):
                        S.op(PE, lambda e, c=c, c4=c4, k=k, wsrc=wsrc: e.matmul(PS[5][:, c4 * 128:(c4 + 1) * 128],
                                                                               lhsT=wsrc[:, k, c * 128:(c + 1) * 128], rhs=hT[:, k, :],
                                                                               start=(k == 0), stop=(k == 7)), [wsrc, hT], [PS[5]])
                if is_silu:
                    S.op(ACT, lambda e: e.activation(out=sig[:, :, :], in_=cols(PS[5][:, 0:512], 4), func=AF.Sigmoid), [PS[5]], [sig])
                    S.op(DVE, lambda e, hh=hh, dst=dst: e.tensor_tensor(out=dst[:, hh * 4:(hh + 1) * 4, :], in0=cols(PS[5][:, 0:512], 4),
                                                                       in1=sig[:, :, :], op=ALU.mult), [PS[5], sig], [dst])
                else:
                    S.op(ACT, lambda e, hh=hh, dst=dst: e.activation(out=dst[:, hh * 4:(hh + 1) * 4, :], in_=cols(PS[5][:, 0:512], 4),
                                                                    func=AF.Sigmoid), [PS[5]], [dst])
        qf = qt[:, :, :].rearrange("p h c -> p (h c)"); kf = kt[:, :, :].rearrange("p h c -> p (h c)")
        for j in range(4):
            S.op(PE, lambda e, j=j: e.transpose(out=PSB[0][:, j * 128:(j + 1) * 128], in_=qf[:, j * 128:(j + 1) * 128],
                                                identity=identb[:, :]), [qt, identb], [PS[0]])
        for j in range(4):
            S.op(PE, lambda e, j=j: e.transpose(out=PSB[0][:, 512 + j * 128:512 + (j + 1) * 128], in_=kf[:, j * 128:(j + 1) * 128],
                                                identity=identb[:, :]), [kt, identb], [PS[0]])
        S.op(ACT, lambda e: e.copy(out=qtT[:, :, :], in_=cols(PSB[0][:, 0:512], 4)), [PS[0]], [qtT])
        S.op(ACT, lambda e: e.copy(out=ktT[:, :, :], in_=cols(PSB[0][:, 512:1024], 4)), [PS[0]], [ktT])
        for h in range(8):
            pr, off = h // 2, (h % 2) * 64
            mb = msc[h % 4]
            ob = PS[6 + h // 4]; oq = h % 4
            S.op(PE, lambda e, h=h, pr=pr, off=off: e.matmul(PS[5][:, (h % 4) * 128:(h % 4 + 1) * 128], lhsT=ktT[off:off + 64, pr, :],
                                                            rhs=qtT[off:off + 64, pr, :], start=True, stop=True),
                 [ktT, qtT], [(PS[5], h % 4)])
            S.op(DVE, lambda e, h=h, mb=mb: e.tensor_tensor(out=mb[:, :], in0=PS[5][:, (h % 4) * 128:(h % 4 + 1) * 128], in1=trib[:, :],
                                                           op=ALU.mult), [(PS[5], h % 4), trib], [mb])
            S.op(PE, lambda e, h=h, mb=mb, ob=ob, oq=oq: e.matmul(ob[:, oq * 128:(oq + 1) * 128], lhsT=vb[:, h, :], rhs=mb[:, :],
                                                                 start=True, stop=False), [vb, mb], [(ob, oq)])
            S.op(PE, lambda e, pr=pr, off=off, ob=ob, oq=oq: e.matmul(ob[:, oq * 128:(oq + 1) * 128], lhsT=Sbf[off:off + 64, pr, :],
                                                                     rhs=qtT[off:off + 64, pr, :], start=False, stop=True),
                 [Sbf, qtT], [(ob, oq)])
        for hh in range(2):
            S.op(ACT, lambda e, hh=hh: e.copy(out=osb[:, hh * 4:(hh + 1) * 4, :], in_=cols(PS[6 + hh][:, 0:512], 4)), [PS[6 + hh]], [osb])
            S.op(ACT, lambda e, hh=hh: e.activation(out=osq[:, hh * 4:(hh + 1) * 4, :], in_=cols(PS[6 + hh][:, 0:512], 4), func=AF.Square),
                 [PS[6 + hh]], [osq])
        state_update()
        S.op(DVE, lambda e: e.tensor_copy(out=Sbf[:, :, :], in_=St[:, :, :]), [St], [Sbf])
        for hh in range(2):
            S.op(PE, lambda e, hh=hh: e.matmul(PS[3 + hh][:, 0:512], lhsT=onesn[:, :],
                                               rhs=osb[:, hh * 4:(hh + 1) * 4, :].rearrange("p h c -> p (h c)"), start=True, stop=True),
                 [onesn, osb], [PS[3 + hh]])
            S.op(PE, lambda e, hh=hh: e.matmul(PS[1 + hh][:, 0:512], lhsT=onesn[:, :],
                                               rhs=osq[:, hh * 4:(hh + 1) * 4, :].rearrange("p h c -> p (h c)"), start=True, stop=True),
                 [onesn, osq], [PS[1 + hh]])
        for hh in range(2):
            sl = slice(hh * 4, (hh + 1) * 4)
            S.op(ACT, lambda e, hh=hh, sl=sl: e.copy(out=mean[:, sl, :], in_=cols(PS[3 + hh][:, 0:512], 4)), [PS[3 + hh]], [mean])
            S.op(DVE, lambda e, sl=sl: e.tensor_tensor(out=var[:, sl, :], in0=mean[:, sl, :], in1=mean[:, sl, :], op=ALU.mult), [mean], [var])
            S.op(DVE, lambda e, hh=hh, sl=sl: e.tensor_tensor(out=var[:, sl, :], in0=cols(PS[1 + hh][:, 0:512], 4), in1=var[:, sl, :],
                                                             op=ALU.subtract), [PS[1 + hh], var], [var])
        S.op(DVE, lambda e: e.tensor_scalar(out=var[:, :, :], in0=var[:, :, :], scalar1=0.0, scalar2=1e-6, op0=ALU.max, op1=ALU.add),
             [var], [var])
        S.op(ACT, lambda e: e.activation(out=var[:, :, :], in_=var[:, :, :], func=AF.Ln), [var], [var])
        S.op(ACT, lambda e: e.activation(out=var[:, :, :], in_=var[:, :, :], func=AF.Exp, scale=-0.5), [var], [var])
        S.op(DVE, lambda e: e.tensor_tensor(out=osb[:, :, :], in0=osb[:, :, :], in1=mean[:, :, :], op=ALU.subtract), [osb, mean], [osb])
        S.op(DVE, lambda e: e.tensor_tensor(out=osb[:, :, :], in0=osb[:, :, :], in1=var[:, :, :], op=ALU.mult), [osb, var], [osb])
        S.op(DVE, lambda e: e.tensor_tensor(out=og[:, :, :], in0=osb[:, :, :], in1=srg[:, :, :], op=ALU.mult), [osb, srg], [og])
        yb_ = ybg[it % 2]
        for c in range(8):
            pbk = PS[3 + c // 4]
            for h in range(8):
                S.op(PE, lambda e, c=c, h=h, pbk=pbk: e.matmul(pbk[:, (c % 4) * 128:(c % 4 + 1) * 128], lhsT=wbb[:, h, c * 128:(c + 1) * 128],
                                                              rhs=og[:, h, :], start=(h == 0), stop=(h == 7)), [wbb, og], [pbk])
        for hh in range(2):
            S.op(DVE, lambda e, hh=hh, yb_=yb_: e.tensor_tensor(out=yb_[:, hh * 4:(hh + 1) * 4, :], in0=cols(PS[3 + hh][:, 0:512], 4),
                                                               in1=sgb[:, hh * 4:(hh + 1) * 4, :], op=ALU.mult), [PS[3 + hh], sgb], [yb_])
        S.dma(SP, lambda e, it=it, yb_=yb_: e.dma_start(out=YB[it].rearrange("p (c t) -> p c t", c=8), in_=yb_[:, :, :]), [yb_], [(YBb, it)])
    A.release(m2)
    S.barrier()

    m3 = A.mark()
    wga = A.alloc([128, 8, 1024], BF16, "wga"); wout = A.alloc([128, 8, 1024], BF16, "wout")
    load_w(wga, win_cols(4616, 5640)); load_w(wout, w_out.ap().rearrange("(k p) c -> p k c", p=128))
    ctx = NormCtx()
    yat = [A.alloc([128, 8, 128], BF16, f"yat{i}") for i in range(2)]
    ybt = [A.alloc([128, 8, 128], BF16, f"ybt{i}") for i in range(2)]
    sga = A.alloc([128, 8, 128], F32, "sga")
    mrg = [A.alloc([128, 8, 128], BF16, f"mrg{i}") for i in range(2)]
    x1t = [A.alloc([128, 1024], F32, f"x1t{i}") for i in range(2)]
    for it in range(NT):
        hT, _h, xt = norm_hT(ctx, xo_v[it], [], 1, 0, 0, want_h=True)
        ya_ = yat[it % 2]; yb_ = ybt[it % 2]; mg = mrg[it % 2]; xo_ = x1t[it % 2]
        S.dma(SP, lambda e, it=it, ya_=ya_: e.dma_start(out=ya_[:, :, :], in_=YA[it].rearrange("p (c t) -> p c t", c=8)), [(YAb, it)], [ya_])
        S.dma(SP, lambda e, it=it, yb_=yb_: e.dma_start(out=yb_[:, :, :], in_=YB[it].rearrange("p (c t) -> p c t", c=8)), [(YBb, it)], [yb_])
        for hh in range(2):
            for c4 in range(4):
                c = hh * 4 + c4
                for k in range(8):
                    S.op(PE, lambda e, c=c, c4=c4, k=k, hh=hh: e.matmul(PS[1 + hh][:, c4 * 128:(c4 + 1) * 128],
                                                                       lhsT=wga[:, k, c * 128:(c + 1) * 128], rhs=hT[:, k, :],
                                                                       start=(k == 0), stop=(k == 7)), [wga, hT], [PS[1 + hh]])
            S.op(ACT, lambda e, hh=hh: e.activation(out=sga[:, hh * 4:(hh + 1) * 4, :], in_=cols(PS[1 + hh][:, 0:512], 4), func=AF.Sigmoid),
                 [PS[1 + hh]], [sga])
        S.op(DVE, lambda e, ya_=ya_: e.tensor_tensor(out=sga[:, :, :], in0=sga[:, :, :], in1=ya_[:, :, :], op=ALU.mult), [sga, ya_], [sga])
        S.op(DVE, lambda e, yb_=yb_, mg=mg: e.tensor_tensor(out=mg[:, :, :], in0=sga[:, :, :], in1=yb_[:, :, :], op=ALU.add), [sga, yb_], [mg])
        for hh in range(2):
            for c in range(8):
                S.op(PE, lambda e, c=c, hh=hh, mg=mg: e.matmul(PS[3 + hh][:, 0:512], lhsT=mg[:, c, :], rhs=wout[:, c, hh * 512:(hh + 1) * 512],
                                                              start=(c == 0), stop=(c == 7)), [mg, wout], [PS[3 + hh]])
            S.op(DVE, lambda e, hh=hh, xo_=xo_: e.tensor_tensor(out=xo_[:, hh * 512:(hh + 1) * 512], in0=PS[3 + hh][:, 0:512],
                                                               in1=mod[:, 2, hh * 512:(hh + 1) * 512], op=ALU.mult),
                 [PS[3 + hh], (mod, 2)], [xo_])
        S.op(DVE, lambda e, xo_=xo_, xt=xt: e.tensor_tensor(out=xo_[:, :], in0=xo_[:, :], in1=xt[:, :], op=ALU.add), [xo_, xt], [xo_])
        S.dma(SP, lambda e, it=it, xo_=xo_: e.dma_start(out=X1[it * 128:(it + 1) * 128, :], in_=xo_[:, :]), [xo_], [(X1b, it)])
    A.release(m3)
    S.barrier()

    CAP = 256; RND = CAP // 8; NSB = CAP // 128; NACC = 4
    H2 = nc.dram_tensor("h2_scr", [2176, 1024], BF16)
    ACCd = [nc.dram_tensor(f"acc_scr{a}", [2176, 1024], F32) for a in range(NACC)]
    H2b = Buf(H2, 17); ACCb = [Buf(ACCd[a], 17) for a in range(NACC)]
    m4 = A.mark()
    h2T = A.alloc([128, 8, 2048], BF16, "h2T", 16)
    Wt = A.alloc([128, 16, 256], F32, "Wt", 16)
    idxT = A.alloc([128, 2, NSB, 128], I32, "idxT"); wT = A.alloc([128, 2, NSB, 128], F32, "wT")
    iot = A.alloc([128, 16], I32, "iot")
    S.dma(SP, lambda e: e.dma_start(out=iot[:, :], in_=iota_d[:, :]), [], [iot])
    mr = A.mark()
    wr = A.alloc([128, 8, 256], BF16, "wr"); rb = A.alloc([128, 256], F32, "rb")
    load_w(wr, w_router.ap().rearrange("(k p) c -> p k c", p=128))
    S.dma(SP, lambda e: e.dma_start(out=rb[:, :], in_=rbias_bc[:, :]), [], [rb])
    zf = A.alloc([128, 1024], F32, "zf"); zb = A.alloc([128, 1024], BF16, "zb")
    S.op(DVE, lambda e: e.memset(zf[:, :], 0.0), [], [zf])
    S.op(DVE, lambda e: e.memset(zb[:, :], 0.0), [], [zb])
    S.dma(SP, lambda e: e.dma_start(out=H2[2048:2176, :], in_=zb[:, :]), [zb], [(H2b, 16)])
    for a in range(NACC):
        for r_ in range(17):
            S.dma(SP, lambda e: e.dma_start(out=ACCd[a][r_ * 128:(r_ + 1) * 128, :], in_=zf[:, :]), [zf], [(ACCb[a], r_)])
    ctx = NormCtx()
    sc = A.alloc([128, 256], F32, "sc"); sel = A.alloc([128, 256], F32, "sel"); selm = A.alloc([128, 256], F32, "selm")
    t8 = A.alloc([128, 8, 8], F32, "t8"); gs = A.alloc([128, 8], F32, "gs"); gt8 = A.alloc([128, 8], F32, "gt8")
    gm = A.alloc([128, 8], F32, "gm"); gp = A.alloc([128, 8], F32, "gp"); e8 = A.alloc([128, 8], F32, "e8")
    em = A.alloc([128, 256], F32, "em"); dn = A.alloc([128, 1], F32, "dn")
    for it in range(NT):
        hT, hh_, _x = norm_hT(ctx, X1[it * 128:(it + 1) * 128, :], [(X1b, it)], 4, 3, 0, want_h=True)
        S.dma(SP, lambda e: e.dma_start(out=H2[it * 128:(it + 1) * 128, :], in_=hh_[:, :]), [hh_], [(H2b, it)])
        S.op(DVE, lambda e: e.tensor_copy(out=h2T[:, :, it * 128:(it + 1) * 128], in_=hT[:, :, :]), [hT], [(h2T, it)])
        for k in range(8):
            S.op(PE, lambda e: e.matmul(PS[1][:, 0:256], lhsT=hT[:, k, :], rhs=wr[:, k, :], start=(k == 0), stop=(k == 7)),
                 [hT, wr], [PS[1]])
        S.op(ACT, lambda e: e.activation(out=sc[:, :], in_=PS[1][:, 0:256], func=AF.Sigmoid), [PS[1]], [sc])
        S.op(DVE, lambda e: e.tensor_tensor(out=sel[:, :], in0=sc[:, :], in1=rb[:, :], op=ALU.add), [sc, rb], [sel])
        for g in range(8):
            S.op(DVE, lambda e: e.max(out=t8[:, g, :], in_=sel[:, g * 32:(g + 1) * 32]), [sel], [t8])
        S.op(DVE, lambda e: e.tensor_tensor(out=gs[:, :], in0=t8[:, :, 0], in1=t8[:, :, 1], op=ALU.add), [t8], [gs])
        S.op(DVE, lambda e: e.max(out=gt8[:, :], in_=gs[:, :]), [gs], [gt8])
        S.op(DVE, lambda e: e.tensor_scalar(out=gm[:, :], in0=gs[:, :], scalar1=gt8[:, 3:4], scalar2=None, op0=ALU.is_ge), [gs, gt8], [gm])
        S.op(DVE, lambda e: e.tensor_scalar(out=gp[:, :], in0=gm[:, :], scalar1=-1.0, scalar2=1.0e4, op0=ALU.add, op1=ALU.mult), [gm], [gp])
        for g in range(8):
            S.op(DVE, lambda e: e.tensor_scalar(out=selm[:, g * 32:(g + 1) * 32], in0=sel[:, g * 32:(g + 1) * 32],
                                                scalar1=gm[:, g:g + 1], scalar2=gp[:, g:g + 1], op0=ALU.mult, op1=ALU.add),
                 [sel, gm, gp], [selm])
        S.op(DVE, lambda e: e.max(out=e8[:, :], in_=selm[:, :]), [selm], [e8])
        S.op(DVE, lambda e: e.tensor_scalar(out=em[:, :], in0=selm[:, :], scalar1=e8[:, 7:8], scalar2=None, op0=ALU.is_ge), [selm, e8], [em])
        S.op(DVE, lambda e: e.tensor_tensor(out=em[:, :], in0=em[:, :], in1=sc[:, :], op=ALU.mult), [em, sc], [em])
        S.op(DVE, lambda e: e.tensor_reduce(out=dn[:, :], in_=em[:, :], axis=AX.X, op=ALU.add), [em], [dn])
        S.op(DVE, lambda e: e.reciprocal(out=dn[:, :], in_=dn[:, :]), [dn], [dn])
        S.op(DVE, lambda e: e.tensor_scalar(out=Wt[:, it, :], in0=em[:, :], scalar1=dn[:, 0:1], scalar2=2.5,
                                            op0=ALU.mult, op1=ALU.mult), [em, dn], [(Wt, it)])
    A.release(mr)
    S.barrier()
    mx = A.mark()
    WT = A.alloc([128, 2, 2048], F32, "WT", 2)
    vals = A.alloc([128, 2, CAP], F32, "vals", 2); idxu = A.alloc([128, 2, CAP], U32, "idxu", 2)
    idxf = A.alloc([128, 2, CAP], F32, "idxf", 2); vld = A.alloc([128, 2, CAP], F32, "vld", 2)
    for hf in range(2):
        for g4 in range(4):
            pb_ = PS[(hf * 4 + g4) % 4]
            for j in range(4):
                it = g4 * 4 + j
                S.op(PE, lambda e: e.matmul(pb_[:, j * 128:(j + 1) * 128], lhsT=Wt[:, it, hf * 128:(hf + 1) * 128], rhs=identf[:, :],
                                            start=True, stop=True), [(Wt, it), identf], [pb_])
            S.op(ACT, lambda e: e.copy(out=WT[:, hf, g4 * 512:(g4 + 1) * 512], in_=pb_[:, 0:512]), [pb_], [(WT, hf)])
    for r_ in range(RND):
        for hf in range(2):
            v8 = vals[:, hf, r_ * 8:(r_ + 1) * 8]
            S.op(DVE, lambda e: e.max(out=v8, in_=WT[:, hf, :]), [(WT, hf)], [(vals, hf)])
            S.op(DVE, lambda e: e.max_index(out=idxu[:, hf, r_ * 8:(r_ + 1) * 8], in_max=v8, in_values=WT[:, hf, :]),
                 [(WT, hf), (vals, hf)], [(idxu, hf)])
            S.op(DVE, lambda e: e.match_replace(out=WT[:, hf, :], in_to_replace=v8, in_values=WT[:, hf, :], imm_value=0.0),
                 [(WT, hf), (vals, hf)], [(WT, hf)])
    S.op(DVE, lambda e: e.tensor_copy(out=idxf[:, :, :], in_=idxu[:, :, :]), [idxu], [idxf])
    S.op(DVE, lambda e: e.tensor_single_scalar(out=vld[:, :, :], in_=vals[:, :, :], scalar=0.0, op=ALU.is_gt), [vals], [vld])
    S.op(DVE, lambda e: e.tensor_scalar(out=idxf[:, :, :], in0=idxf[:, :, :], scalar1=-2048.0, scalar2=None, op0=ALU.add), [idxf], [idxf])
    S.op(DVE, lambda e: e.tensor_tensor(out=idxf[:, :, :], in0=idxf[:, :, :], in1=vld[:, :, :], op=ALU.mult), [idxf, vld], [idxf])
    S.op(DVE, lambda e: e.tensor_scalar(out=idxf[:, :, :], in0=idxf[:, :, :], scalar1=2048.25, scalar2=None, op0=ALU.add), [idxf], [idxf])
    for hf in range(2):
        for sb_ in range(NSB):
            q_ = hf * NSB + sb_
            S.op(PE, lambda e: e.matmul(PS[4][:, q_ * 128:(q_ + 1) * 128], lhsT=idxf[:, hf, sb_ * 128:(sb_ + 1) * 128], rhs=identf[:, :],
                                        start=True, stop=True), [idxf, identf], [PS[4]])
            S.op(PE, lambda e: e.matmul(PS[5][:, q_ * 128:(q_ + 1) * 128], lhsT=vals[:, hf, sb_ * 128:(sb_ + 1) * 128], rhs=identf[:, :],
                                        start=True, stop=True), [vals, identf], [PS[5]])
    S.op(DVE, lambda e: e.tensor_copy(out=idxT[:, :, :, :].rearrange("p a b c -> p (a b c)"), in_=PS[4][:, 0:2 * NSB * 128]), [PS[4]], [idxT])
    S.op(ACT, lambda e: e.copy(out=wT[:, :, :, :].rearrange("p a b c -> p (a b c)"), in_=PS[5][:, 0:2 * NSB * 128]), [PS[5]], [wT])
    A.release(mx)
    S.barrier()
    NWS = 3
    _bc = {}

    def bc_reg(e):
        if "r" not in _bc:
            r_ = e.alloc_register("bcreg")
            e.reg_mov(r_, 2175)
            _bc["r"] = r_
        return _bc["r"]

    wgu = [A.alloc([128, 8, 512], BF16, f"wgu{i}") for i in range(NWS)]
    wdn = [A.alloc([128, 2, 1024], BF16, f"wdn{i}") for i in range(NWS)]
    Xg = [A.alloc([128, 1024], BF16, f"Xg{i}") for i in range(4)]
    XT = [A.alloc([128, 8, 128], BF16, f"XT{i}") for i in range(3)]
    sgm = [A.alloc([128, 256], F32, f"sgm{i}") for i in range(2)]
    tg = [A.alloc([128, 256], F32, f"tg{i}") for i in range(2)]
    Ab = [A.alloc([128, 256], BF16, f"Ab{i}") for i in range(2)]
    ATb = [A.alloc([128, 2, 128], BF16, f"ATb{i}") for i in range(2)]
    Yb = [A.alloc([128, 1024], F32, f"Yb{i}") for i in range(2)]
    blocks = []
    for ex in range(256):
        hf, el = ex // 128, ex % 128
        for sb_ in range(NSB):
            blocks.append((ex, idxT[:, hf, sb_, el:el + 1], wT[:, hf, sb_, el:el + 1], None))
    for it in range(NT):
        blocks.append((256, iot[:, it:it + 1], onesf[:, 0:1], it))
    NB = len(blocks)

    stg_g = [A.alloc([128, 8, 256], F32, f"stg_g{i}") for i in range(2)]
    stg_u = [A.alloc([128, 8, 256], F32, f"stg_u{i}") for i in range(2)]
    stg_dn = [A.alloc([128, 2, 1024], F32, f"stg_dn{i}") for i in range(2)]

    def stage_expert(ex):
        st_ = ex % 2
        if ex < 256:
            g_ap = w_eg.ap()[ex].rearrange("(p k) c -> p k c", k=8)
            u_ap = w_eu.ap()[ex].rearrange("(p k) c -> p k c", k=8)
            d_ap = w_ed.ap()[ex].rearrange("(p k) c -> p k c", k=2)
        else:
            g_ap = w_sg.ap().rearrange("(k p) c -> p k c", p=128)
            u_ap = w_su.ap().rearrange("(k p) c -> p k c", p=128)
            d_ap = w_sd.ap().rearrange("(k p) c -> p k c", p=128)
        S.dma(SP, lambda e: e.dma_start(out=stg_g[st_][:, :, :], in_=g_ap), [], [stg_g[st_]])
        S.dma(SP, lambda e: e.dma_start(out=stg_u[st_][:, :, :], in_=u_ap), [], [stg_u[st_]])
        S.dma(SP, lambda e: e.dma_start(out=stg_dn[st_][:, :, :], in_=d_ap), [], [stg_dn[st_]])

    def cast_expert(ex):
        st_ = ex % 2; sl = ex % NWS
        S.op(ACT, lambda e: e.copy(out=wgu[sl][:, :, 0:256], in_=stg_g[st_][:, :, :]), [stg_g[st_]], [wgu[sl]])
        S.op(ACT, lambda e: e.copy(out=wgu[sl][:, :, 256:512], in_=stg_u[st_][:, :, :]), [stg_u[st_]], [wgu[sl]])
        S.op(DVE, lambda e: e.tensor_copy(out=wdn[sl][:, :, :], in_=stg_dn[st_][:, :, :]), [stg_dn[st_]], [wdn[sl]])

    def st_gather(b):
        ex, ic, wc, sh = blocks[b]
        if sh is None:
            S.dma(POOL, lambda e: e.indirect_dma_start(out=Xg[b % 4][:, :], out_offset=None, in_=H2[:, :],
                                                       in_offset=bass.IndirectOffsetOnAxis(ap=ic, axis=0),
                                                       bounds_check=bc_reg(e), oob_is_err=False), [idxT, H2b], [Xg[b % 4]])

    def st_T(b):
        ex, ic, wc, sh = blocks[b]
        if sh is not None:
            return
        pT = 0 if b % 2 == 0 else 3
        for k in range(8):
            S.op(PE, lambda e: e.transpose(out=PSB[pT][:, k * 128:(k + 1) * 128],
                                           in_=Xg[b % 4][:, :].rearrange("s (p k) -> s k p", k=8)[:, k, :],
                                           identity=identb[:, :]), [Xg[b % 4], identb], [PS[pT]])
        S.op(ACT, lambda e: e.copy(out=XT[b % 3][:, :, :], in_=cols(PSB[pT][:, 0:1024], 8)), [PS[pT]], [XT[b % 3]])

    def st_M1(b):
        ex, ic, wc, sh = blocks[b]
        gu = PS[1 + b % 2]
        for k in range(8):
            if sh is None:
                lh = XT[b % 3][:, k, :]; rd_ = [XT[b % 3]]
            else:
                lh = h2T[:, k, sh * 128:(sh + 1) * 128]; rd_ = [(h2T, sh)]
            S.op(PE, lambda e: e.matmul(gu[:, 0:512], lhsT=lh, rhs=wgu[ex % NWS][:, k, :], start=(k == 0), stop=(k == 7)),
                 rd_ + [wgu[ex % NWS]], [gu])
        sg_ = sgm[b % 2]; tg_ = tg[b % 2]; a_ = Ab[b % 2]
        S.op(ACT, lambda e: e.activation(out=sg_[:, :], in_=gu[:, 0:256], func=AF.Sigmoid), [gu], [sg_])
        S.op(DVE, lambda e: e.tensor_tensor(out=tg_[:, :], in0=gu[:, 0:256], in1=sg_[:, :], op=ALU.mult), [gu, sg_], [tg_])
        S.op(DVE, lambda e: e.scalar_tensor_tensor(out=a_[:, :], in0=tg_[:, :], scalar=wc, in1=gu[:, 256:512],
                                                   op0=ALU.mult, op1=ALU.mult), [gu, tg_, wT, onesf], [a_])

    def st_M2(b):
        ex, ic, wc, sh = blocks[b]
        a_ = Ab[b % 2]; at_ = ATb[b % 2]; y_ = Yb[b % 2]
        for c in range(2):
            a_in = a_[:, c * 128:(c + 1) * 128] if sh is not None else a_[:, :].rearrange("s (p c) -> s c p", c=2)[:, c, :]
            S.op(PE, lambda e: e.transpose(out=PSB[5][:, c * 128:(c + 1) * 128], in_=a_in,
                                           identity=identb[:, :]), [a_, identb], [PS[5]])
        S.op(ACT, lambda e: e.copy(out=at_[:, :, :], in_=cols(PSB[5][:, 0:256], 2)), [PS[5]], [at_])
        for hh in range(2):
            for c in range(2):
                S.op(PE, lambda e: e.matmul(PS[6 + hh][:, 0:512], lhsT=at_[:, c, :], rhs=wdn[ex % NWS][:, c, hh * 512:(hh + 1) * 512],
                                            start=(c == 0), stop=(c == 1)), [at_, wdn[ex % NWS]], [PS[6 + hh]])
        S.op(ACT, lambda e: e.copy(out=y_[:, 0:512], in_=PS[6][:, 0:512]), [PS[6]], [y_])
        S.op(DVE, lambda e: e.tensor_copy(out=y_[:, 512:1024], in_=PS[7][:, 0:512]), [PS[7]], [y_])
        S.dma(POOL, lambda e: e.indirect_dma_start(out=ACCd[b % NACC][:, :], out_offset=bass.IndirectOffsetOnAxis(ap=ic, axis=0),
                                                   in_=y_[:, :], in_offset=None, bounds_check=bc_reg(e), oob_is_err=False,
                                                   compute_op=ALU.add),
              [y_, idxT, iot], [ACCb[b % NACC]])

    stage_expert(0); stage_expert(1)
    cast_expert(0)
    stage_expert(2)
    cast_expert(1)
    for b in range(3):
        st_gather(b)
    st_T(0); st_T(1)
    for b in range(NB):
        ex = blocks[b][0]
        if b + 3 < NB:
            st_gather(b + 3)
        st_M1(b)
        if b + 2 < NB:
            st_T(b + 2)
        if b >= 1:
            st_M2(b - 1)
        if b == 0 or blocks[b - 1][0] != ex:
            if ex + 2 <= 256:
                cast_expert(ex + 2)
            if ex + 3 <= 256:
                stage_expert(ex + 3)
    st_M2(NB - 1)
    A.release(m4)
    S.barrier()
    xf = [A.alloc([128, 1024], F32, f"xf{i}") for i in range(2)]
    af = [[A.alloc([128, 1024], F32, f"af{i}{a}") for a in range(NACC)] for i in range(2)]
    fj = A.alloc([128, 1024], BF16, "fj")
    fss = [A.alloc([128, 1], F32, f"fss{i}") for i in range(2)]; frs = [A.alloc([128, 1], F32, f"frs{i}") for i in range(2)]
    for it in range(NT):
        x_ = xf[it % 2]; ss_ = fss[it % 2]; rs_ = frs[it % 2]; af_ = af[it % 2]
        S.dma(SP, lambda e: e.dma_start(out=x_[:, :], in_=X1[it * 128:(it + 1) * 128, :]), [(X1b, it)], [x_])
        for a in range(NACC):
            S.dma(SP, lambda e: e.dma_start(out=af_[a][:, :], in_=ACCd[a][it * 128:(it + 1) * 128, :]), [ACCb[a]], [af_[a]])
        S.op(DVE, lambda e: e.tensor_tensor(out=af_[0][:, :], in0=af_[0][:, :], in1=af_[1][:, :], op=ALU.add), [af_[0], af_[1]], [af_[0]])
        S.op(DVE, lambda e: e.tensor_tensor(out=af_[2][:, :], in0=af_[2][:, :], in1=af_[3][:, :], op=ALU.add), [af_[2], af_[3]], [af_[2]])
        S.op(DVE, lambda e: e.tensor_tensor(out=af_[0][:, :], in0=af_[0][:, :], in1=af_[2][:, :], op=ALU.add), [af_[0], af_[2]], [af_[0]])
        S.op(DVE, lambda e: e.tensor_tensor(out=af_[0][:, :], in0=af_[0][:, :], in1=mod[:, 5, :], op=ALU.mult), [af_[0], (mod, 5)], [af_[0]])
        S.op(DVE, lambda e: e.tensor_tensor(out=x_[:, :], in0=x_[:, :], in1=af_[0][:, :], op=ALU.add), [x_, af_[0]], [x_])
        rstd_of(ss_, rs_, x_[:, :], x_, fj)
        S.op(DVE, lambda e: e.scalar_tensor_tensor(out=x_[:, :], in0=x_[:, :], scalar=rs_[:, 0:1], in1=fgt[:, :],
                                                   op0=ALU.mult, op1=ALU.mult), [x_, rs_, fgt], [x_])
        S.dma(SP, lambda e: e.dma_start(out=out_d[it * 128:(it + 1) * 128, :], in_=x_[:, :]), [x_], [])
    S.analyze()
    S.emit()
    return nc


_CONST_CACHE = {}


def _constants():
    if _CONST_CACHE:
        return _CONST_CACHE
    ident = np.eye(128, dtype=np.float32)
    tri = (np.arange(128)[:, None] <= np.arange(128)[None, :]).astype(np.float32)
    H = 8
    gamma = 1.0 - np.exp2(-5.0 - np.arange(H, dtype=np.float64))
    half = 32
    inv_freq = (np.float32(10000.0) ** (-(np.arange(half, dtype=np.float32) / np.float32(half)))).astype(np.float32)
    pos = np.arange(4096, dtype=np.float32)
    ang = (pos[:, None] * inv_freq[None, :]).astype(np.float32).astype(np.float64)
    cos, sin = np.cos(ang), np.sin(ang)
    p = (np.arange(4096) % 128).astype(np.float64)
    dq = gamma[None, :] ** (p[:, None] + 1.0)
    dk = gamma[None, :] ** (-(p[:, None] + 1.0)) * (64 ** -0.5)
    def tab(cs, d):
        return (cs[:, None, :] * d[:, :, None]).reshape(4096, 256).astype(np.float32)
    _CONST_CACHE.update(
        ident=ident, tri=tri,
        k_c=tab(cos, dk), k_s=tab(sin, dk), q_c=tab(cos, dq), q_s=tab(sin, dq))
    cd = gamma ** 128.0
    cdt = np.zeros((128, 4, 128), np.float32)
    for m in range(4):
        cdt[0:64, m, :] = cd[2 * m]
        cdt[64:128, m, :] = cd[2 * m + 1]
    _CONST_CACHE["cdt"] = cdt.reshape(128, 512)
    return _CONST_CACHE


_NC_CACHE = {}


def kernel(x, c, w_ada, b_ada, norm1_g, w_in, b_forget, ret_gn_g, w_branch_a, w_branch_b, w_out, norm2_g,
           w_router, router_bias, w_exp_gate, w_exp_up, w_exp_down, w_sh_gate, w_sh_up, w_sh_down, final_g):
    f32 = lambda a: np.ascontiguousarray(np.asarray(a, dtype=np.float32))
    x = f32(x); c = f32(c)
    K = _constants()
    bc = lambda v: np.ascontiguousarray(np.broadcast_to(f32(v).reshape(1, -1), (128, f32(v).size)))
    shared = {
        "w_ada": f32(w_ada)[0], "b_ada_bc": bc(f32(b_ada)[0]),
        "g1_bc": bc(f32(norm1_g)[0]), "g2_bc": bc(f32(norm2_g)[0]), "fg_bc": bc(f32(final_g)),
        "w_in": f32(w_in)[0], "bfg_bc": bc(f32(b_forget)[0]),
        "gng": np.ascontiguousarray(f32(ret_gn_g)[0].reshape(8, 128).T),
        "w_ba": f32(w_branch_a)[0], "w_bb": f32(w_branch_b)[0], "w_out": f32(w_out)[0],
        "w_router": f32(w_router)[0], "rbias_bc": bc(f32(router_bias)[0]),
        "w_eg": f32(w_exp_gate)[0], "w_eu": f32(w_exp_up)[0], "w_ed": f32(w_exp_down)[0],
        "w_sg": f32(w_sh_gate)[0], "w_su": f32(w_sh_up)[0], "w_sd": f32(w_sh_down)[0],
        "ident": K["ident"], "tri": K["tri"], "cdt": K["cdt"],
        "iota": (np.arange(16, dtype=np.int32)[None, :] * 128 + np.arange(128, dtype=np.int32)[:, None]).astype(np.int32),
    }
    in_maps = []
    for core in range(8):
        b, half = core // 2, core % 2
        lo = half * 2048
        m = dict(shared)
        m["xo"] = np.ascontiguousarray(x[b, lo:lo + 2048])
        m["xc"] = np.ascontiguousarray(x[b, 0:2048])
        m["cin"] = np.ascontiguousarray(c[b].reshape(8, 128).T)
        m["flag"] = np.full((128, 1), float(half), np.float32)
        m["rkc_c"] = K["k_c"][0:2048]; m["rkc_s"] = K["k_s"][0:2048]
        m["rko_c"] = K["k_c"][lo:lo + 2048]; m["rko_s"] = K["k_s"][lo:lo + 2048]
        m["rqo_c"] = K["q_c"][lo:lo + 2048]; m["rqo_s"] = K["q_s"][lo:lo + 2048]
        in_maps.append(m)
    if "nc" not in _NC_CACHE:
        _NC_CACHE["nc"] = build_program()
    res = run_bass_kernel_spmd(_NC_CACHE["nc"], in_maps, core_ids=list(range(8)))
    out = np.empty((4, 4096, 1024), np.float32)
    for core in range(8):
        b, half = core // 2, core % 2
        out[b, half * 2048:(half + 1) * 2048] = res.results[core]["out"]
    return out
```

```python
from concourse.bass_utils import run_bass_kernel_spmd
import concourse.bass as bass
import concourse.mybir as mybir

F32 = mybir.dt.float32
BF16 = mybir.dt.bfloat16
I32 = mybir.dt.int32
U32 = mybir.dt.uint32
U8 = mybir.dt.uint8
ALU = mybir.AluOpType
AF = mybir.ActivationFunctionType
AX = mybir.AxisListType

PE, ACT, DVE, POOL, SP = "pe", "act", "dve", "pool", "sp"
COMPUTE = (PE, ACT, DVE, POOL)


class Buf:
    _n = 0

    def __init__(self, t, nparts=1, name=None):
        self.t = t
        self.nparts = nparts
        Buf._n += 1
        self.id = Buf._n
        self.name = name

    def keys(self, parts=None):
        if parts is None:
            return [(self.id, p) for p in range(self.nparts)]
        if isinstance(parts, int):
            return [(self.id, parts)]
        return [(self.id, p) for p in parts]

    def __getitem__(self, idx):
        return self.t[idx]


class Acc:
    def __init__(self, buf, parts=None):
        self.buf = buf
        self.parts = parts

    def keys(self):
        return self.buf.keys(self.parts)


def _keys(lst):
    out = []
    for a in lst:
        if a is None:
            continue
        if isinstance(a, Buf):
            out.extend(a.keys())
        elif isinstance(a, Acc):
            out.extend(a.keys())
        elif isinstance(a, tuple) and isinstance(a[0], Buf):
            out.extend(a[0].keys(a[1]))
        else:
            raise TypeError(a)
    return out


class Op:
    __slots__ = ("eng", "fn", "rk", "wk", "dma", "idx", "eidx", "waits", "signal",
                 "sem", "semval", "barrier", "deps", "sigidx")

    def __init__(self, eng, fn, rk, wk, dma):
        self.eng = eng
        self.fn = fn
        self.rk = rk
        self.wk = wk
        self.dma = dma
        self.waits = []
        self.signal = False
        self.sem = None
        self.semval = None
        self.barrier = False
        self.deps = None


import types


def _freeze(fn):
    if fn.__closure__ is None:
        return fn
    cells = []
    for c in fn.__closure__:
        try:
            cells.append(types.CellType(c.cell_contents))
        except ValueError:
            cells.append(c)
    g = types.FunctionType(fn.__code__, fn.__globals__, fn.__name__, fn.__defaults__, tuple(cells))
    g.__kwdefaults__ = fn.__kwdefaults__
    return g


class Sched:
    SEM_EPOCH = 20000
    N_DMA_SEMS_SP = 24
    N_DMA_SEMS_POOL = 40

    def __init__(self, nc):
        self.nc = nc
        self.ops = []

    def op(self, eng, fn, reads=(), writes=()):
        o = Op(eng, _freeze(fn), _keys(reads), _keys(writes), False)
        o.idx = len(self.ops)
        self.ops.append(o)
        return o

    def dma(self, eng, fn, reads=(), writes=()):
        o = Op(eng, _freeze(fn), _keys(reads), _keys(writes), True)
        o.idx = len(self.ops)
        self.ops.append(o)
        return o

    def barrier(self):
        o = Op(None, None, [], [], False)
        o.barrier = True
        o.idx = len(self.ops)
        self.ops.append(o)

    def analyze(self):
        last_w = {}
        readers = {}
        engs = (PE, ACT, DVE, POOL, SP)
        eng_ops = {e: [] for e in engs}
        last_on_eng = {e: None for e in engs}
        dma_since_barrier = []
        pending_front = {e: [] for e in engs}
        ops = self.ops
        for o in ops:
            if o.barrier:
                front = [v for e, v in last_on_eng.items() if v is not None and e in COMPUTE]
                front += dma_since_barrier
                dma_since_barrier = []
                for e in engs:
                    pending_front[e] = list(front)
                last_w.clear()
                readers.clear()
                continue
            deps = set()
            for k in o.rk:
                w = last_w.get(k)
                if w is not None:
                    deps.add((w, "raw"))
            for k in o.wk:
                w = last_w.get(k)
                if w is not None:
                    deps.add((w, "waw"))
                for r in readers.get(k, ()):
                    deps.add((r, "war"))
            if pending_front[o.eng]:
                for f in pending_front[o.eng]:
                    deps.add((f, "bar"))
                pending_front[o.eng] = []
            o.deps = deps
            for k in o.rk:
                readers.setdefault(k, []).append(o.idx)
            for k in o.wk:
                last_w[k] = o.idx
                readers[k] = []
            o.eidx = len(eng_ops[o.eng])
            eng_ops[o.eng].append(o)
            if o.dma:
                dma_since_barrier.append(o.idx)
            else:
                last_on_eng[o.eng] = o.idx
        self.eng_ops = eng_ops
        for o in ops:
            if o.barrier:
                continue
            per_eng = {}
            dma_deps = set()
            for (d, kind) in o.deps:
                if d == o.idx:
                    continue
                p = ops[d]
                if p.dma:
                    dma_deps.add(d)
                    continue
                if p.eng == o.eng and not o.dma:
                    if o.eng == PE:
                        continue
                    if kind not in ("raw", "war", "waw"):
                        continue
                    if o.eidx - p.eidx > 3:
                        continue
                prev = per_eng.get(p.eng)
                if prev is None or p.eidx > ops[prev].eidx:
                    per_eng[p.eng] = d
            o.waits = list(per_eng.values()) + sorted(dma_deps)
            for d in o.waits:
                ops[d].signal = True
        for o in ops:
            if not o.barrier and o.dma:
                o.signal = True

    def emit(self, final_wait_engine=SP):
        nc = self.nc
        ops = self.ops
        import contextlib
        self._stack = contextlib.ExitStack()
        st = self._stack
        eng_sems = {}
        for e in COMPUTE:
            nsig = sum(1 for o in self.eng_ops[e] if o.signal and not o.dma)
            nep = nsig // self.SEM_EPOCH + 1
            eng_sems[e] = [st.enter_context(nc.semaphore(f"s_{e}_{i}")) for i in range(nep)]
            c = 0
            for o in self.eng_ops[e]:
                if o.signal and not o.dma:
                    o.sem = eng_sems[e][c // self.SEM_EPOCH]
                    o.semval = c % self.SEM_EPOCH + 1
                    o.sigidx = c
                    c += 1
        pools = {}
        for qn, nsem in ((SP, self.N_DMA_SEMS_SP), (POOL, self.N_DMA_SEMS_POOL), (ACT, 4)):
            if any(o.dma and o.eng == qn for o in ops if not o.barrier):
                pools[qn] = dict(sems=[st.enter_context(nc.semaphore(f"s_dma_{qn}_{i}")) for i in range(nsem)],
                                 counts=[0] * nsem, last=[None] * nsem, di=0)
        for o in ops:
            if o.barrier or not o.dma:
                continue
            P = pools[o.eng]
            n = len(P["sems"])
            s = P["di"] % n
            P["di"] += 1
            prev = P["last"][s]
            if prev is not None and prev not in o.waits:
                o.waits.append(prev)
            P["counts"][s] += 1
            o.sem = P["sems"][s]
            o.semval = 16 * P["counts"][s]
            P["last"][s] = o.idx
            if o.semval > 60000:
                raise RuntimeError("dma sem overflow")
        self.n_wait_insts = 0

        def run_engine(ename, eng):
            known_eng = {}
            known_dma = {}
            for o in self.eng_ops[ename]:
                for d in o.waits:
                    p = ops[d]
                    if p.dma:
                        key = id(p.sem)
                        if known_dma.get(key, 0) >= p.semval:
                            continue
                        known_dma[key] = p.semval
                        eng.wait_ge(p.sem, p.semval)
                        self.n_wait_insts += 1
                    else:
                        if known_eng.get(p.eng, -1) >= p.sigidx:
                            continue
                        known_eng[p.eng] = p.sigidx
                        eng.wait_ge(p.sem, p.semval)
                        self.n_wait_insts += 1
                ins = o.fn(eng)
                if o.signal:
                    ins.then_inc(o.sem, 16 if o.dma else 1)
            return known_eng, known_dma

        last_dma = [o for o in ops if not o.barrier and o.dma]
        with nc.Block() as block:
            @block.tensor
            def _(e):
                run_engine(PE, e)

            @block.scalar
            def _(e):
                run_engine(ACT, e)

            @block.vector
            def _(e):
                run_engine(DVE, e)

            @block.gpsimd
            def _(e):
                run_engine(POOL, e)

            @block.sync
            def _(e):
                ke, kd = run_engine(SP, e)
                finals = {}
                for o in last_dma:
                    key = id(o.sem)
                    if finals.get(key, (None, 0))[1] < o.semval:
                        finals[key] = (o.sem, o.semval)
                for sem, val in finals.values():
                    e.wait_ge(sem, val)
                for en in COMPUTE:
                    sig = [o for o in self.eng_ops[en] if o.signal and not o.dma]
                    if sig:
                        e.wait_ge(sig[-1].sem, sig[-1].semval)
        st.close()


class Arena:
    def __init__(self, nc, base, limit):
        self.nc = nc
        self.base = base
        self.top = base
        self.limit = limit
        self.n = 0

    def alloc(self, shape, dtype, name=None, nparts=1):
        esz = {F32: 4, BF16: 2, I32: 4, U32: 4, U8: 1}[dtype]
        n = 1
        for s in shape[1:]:
            n *= s
        nbytes = (n * esz + 63) // 64 * 64
        off = self.top
        if off + nbytes > self.limit:
            raise MemoryError(f"SBUF arena overflow: {name} needs {nbytes}, top={off}, limit={self.limit}")
        self.top += nbytes
        self.n += 1
        t = self.nc.alloc_sbuf_tensor_at(f"{name or 't'}_{self.n}", list(shape), dtype, offset=off)
        return Buf(t, nparts, name)

    def mark(self):
        return self.top

    def release(self, m):
        self.top = m


import numpy as np

NT = 16
NE = 256
CAPW = 257


def build_program():
    nc = bass.Bass("TRN2", target_bir_lowering=False)

    def din(name, shape, dt=F32):
        return nc.dram_tensor(name, list(shape), dt, kind="ExternalInput")

    xo = din("xo", [2048, 1024]); xc = din("xc", [2048, 1024])
    cin = din("cin", [128, 8]); flag_d = din("flag", [128, 1])
    w_ada = din("w_ada", [1024, 6144]); b_ada_bc = din("b_ada_bc", [128, 6144])
    g1_bc = din("g1_bc", [128, 1024]); g2_bc = din("g2_bc", [128, 1024]); fg_bc = din("fg_bc", [128, 1024])
    w_in = din("w_in", [1024, 6664]); bfg_bc = din("bfg_bc", [128, 8]); gng_d = din("gng", [128, 8])
    w_ba = din("w_ba", [512, 1024]); w_bb = din("w_bb", [1024, 1024]); w_out = din("w_out", [1024, 1024])
    w_router = din("w_router", [1024, 256]); rbias_bc = din("rbias_bc", [128, 256])
    w_eg = din("w_eg", [256, 1024, 256]); w_eu = din("w_eu", [256, 1024, 256]); w_ed = din("w_ed", [256, 256, 1024])
    w_sg = din("w_sg", [1024, 256]); w_su = din("w_su", [1024, 256]); w_sd = din("w_sd", [256, 1024])
    ident_d = din("ident", [128, 128]); tri_d = din("tri", [128, 128])
    rot = {k: din(k, [2048, 256]) for k in ("rkc_c", "rkc_s", "rko_c", "rko_s", "rqo_c", "rqo_s")}
    cdt_d = din("cdt", [128, 512])
    iota_d = din("iota", [128, 16], I32)
    dummy_d = din("dummyrow", [128, 2 * 256])
    out_d = nc.dram_tensor("out", [2048, 1024], F32, kind="ExternalOutput")
    YA = nc.dram_tensor("ya_scr", [16, 128, 1024], BF16)
    X1 = nc.dram_tensor("x1_scr", [2048, 1024], F32)
    YAb = Buf(YA, 16); X1b = Buf(X1, 16)

    ARENA_BYTES = 210000
    arena_t = nc.alloc_sbuf_tensor("arena", [128, ARENA_BYTES], U8)
    base = nc.lookup_mloc(arena_t).addr
    A = Arena(nc, base, base + ARENA_BYTES)
    S = Sched(nc)
    PS = [Buf(nc.alloc_psum_tensor(f"psb{i}", [128, 512], F32), 4, f"ps{i}") for i in range(8)]
    PSB = [p.t.bitcast(BF16) for p in PS]

    def cols(ap2d, a):
        return ap2d.rearrange("p (a c) -> p a c", a=a)

    identf = A.alloc([128, 128], F32, "identf"); identb = A.alloc([128, 128], BF16, "identb")
    trif = A.alloc([128, 128], F32, "trif"); trib = A.alloc([128, 128], BF16, "trib")
    onesf = A.alloc([128, 128], F32, "onesf")
    flag = A.alloc([128, 1], F32, "flag"); omf = A.alloc([128, 1], F32, "omf")
    mod = A.alloc([128, 6, 1024], F32, "mod", 6)
    fgt = A.alloc([128, 1024], F32, "fgt")
    S.dma(SP, lambda e: e.dma_start(out=identf[:, :], in_=ident_d[:, :]), [], [identf])
    S.dma(SP, lambda e: e.dma_start(out=trif[:, :], in_=tri_d[:, :]), [], [trif])
    S.dma(SP, lambda e: e.dma_start(out=flag[:, :], in_=flag_d[:, :]), [], [flag])
    S.dma(SP, lambda e: e.dma_start(out=fgt[:, :], in_=fg_bc[:, :]), [], [fgt])
    S.op(DVE, lambda e: e.tensor_copy(out=identb[:, :], in_=identf[:, :]), [identf], [identb])
    S.op(DVE, lambda e: e.tensor_copy(out=trib[:, :], in_=trif[:, :]), [trif], [trib])
    S.op(DVE, lambda e: e.memset(onesf[:, :], 1.0), [], [onesf])
    S.op(DVE, lambda e: e.tensor_scalar(out=omf[:, :], in0=flag[:, :], scalar1=-1.0, scalar2=30000.0,
                                        op0=ALU.add, op1=ALU.mult), [flag], [omf])

    mA = A.mark()
    cs = A.alloc([128, 8], F32, "cs"); csg = A.alloc([128, 8], F32, "csg")
    scb = A.alloc([128, 8, 128], F32, "scb")
    wada = [A.alloc([128, 8, 512], F32, f"wada{i}") for i in range(2)]
    btmp = [A.alloc([128, 512], F32, f"btmp{i}") for i in range(2)]
    gtmp = A.alloc([128, 1024], F32, "gtmp")
    S.dma(SP, lambda e: e.dma_start(out=cs[:, :], in_=cin[:, :]), [], [cs])
    S.op(ACT, lambda e: e.activation(out=csg[:, :], in_=cs[:, :], func=AF.Sigmoid), [cs], [csg])
    S.op(DVE, lambda e: e.tensor_tensor(out=csg[:, :], in0=csg[:, :], in1=cs[:, :], op=ALU.mult), [csg, cs], [csg])
    for k in range(8):
        S.op(DVE, lambda e, k=k: e.tensor_scalar(out=scb[:, k, :], in0=onesf[:, :], scalar1=csg[:, k:k + 1], scalar2=None,
                                                 op0=ALU.mult), [onesf, csg], [scb])
    wada_v = w_ada.ap().rearrange("(k p) c -> p k c", p=128)
    for j in range(12):
        wt = wada[j % 2]; bt = btmp[j % 2]; ps = PS[j % 2]
        S.dma(SP, lambda e, wt=wt, j=j: e.dma_start(out=wt[:, :, :], in_=wada_v[:, :, j * 512:(j + 1) * 512]), [], [wt])
        S.dma(SP, lambda e, bt=bt, j=j: e.dma_start(out=bt[:, :], in_=b_ada_bc[:, j * 512:(j + 1) * 512]), [], [bt])
        for k in range(8):
            S.op(PE, lambda e, ps=ps, wt=wt, k=k: e.matmul(ps[:, 0:512], lhsT=scb[:, k, :], rhs=wt[:, k, :],
                                                          start=(k == 0), stop=(k == 7)), [scb, wt], [ps])
        S.op(DVE, lambda e, ps=ps, bt=bt, j=j: e.tensor_tensor(out=mod[:, j // 2, (j % 2) * 512:(j % 2 + 1) * 512],
                                                               in0=ps[:, 0:512], in1=bt[:, :], op=ALU.add),
             [ps, bt], [(mod, j // 2)])
    for (slot, gsrc) in ((1, g1_bc), (4, g2_bc)):
        S.dma(SP, lambda e, gsrc=gsrc: e.dma_start(out=gtmp[:, :], in_=gsrc[:, :]), [], [gtmp])
        S.op(DVE, lambda e, slot=slot: e.scalar_tensor_tensor(out=mod[:, slot, :], in0=mod[:, slot, :], scalar=1.0,
                                                              in1=gtmp[:, :], op0=ALU.add, op1=ALU.mult),
             [(mod, slot), gtmp], [(mod, slot)])
    A.release(mA)
    S.barrier()

    def load_w(dst, src_ap):
        S.dma(POOL, lambda e: e.dma_start(out=dst[:, :, :], in_=src_ap), [], [dst])

    def win_cols(c0, c1):
        return w_in.ap()[:, c0:c1].rearrange("(k p) c -> p k c", p=128)

    class NormCtx:
        def __init__(self):
            self.x = [A.alloc([128, 1024], F32, f"nx{i}") for i in range(2)]
            self.junk = A.alloc([128, 1024], BF16, "njunk")
            self.t = A.alloc([128, 1024], F32, "nt")
            self.h = [A.alloc([128, 1024], BF16, f"nh{i}") for i in range(2)]
            self.hT = [A.alloc([128, 8, 128], BF16, f"nhT{i}") for i in range(2)]
            self.ss = [A.alloc([128, 1], F32, f"nss{i}") for i in range(2)]
            self.rs = [A.alloc([128, 1], F32, f"nrs{i}") for i in range(2)]
            self.n = 0

    def rstd_of(ss, rs, x_ap, xbuf, junk):
        S.op(DVE, lambda e: e.memset(ss[:, :], 0.0), [], [ss])
        S.op(ACT, lambda e: e.activation(out=junk[:, :], in_=x_ap, func=AF.Square, accum_out=ss[:, 0:1]), [xbuf], [junk, ss])
        S.op(DVE, lambda e: e.tensor_scalar(out=rs[:, :], in0=ss[:, :], scalar1=1.0 / 1024, scalar2=1e-6,
                                            op0=ALU.mult, op1=ALU.add), [ss], [rs])
        S.op(ACT, lambda e: e.activation(out=rs[:, :], in_=rs[:, :], func=AF.Ln), [rs], [rs])
        S.op(ACT, lambda e: e.activation(out=rs[:, :], in_=rs[:, :], func=AF.Exp, scale=-0.5), [rs], [rs])

    def norm_hT(ctx, src_rows_ap, src_dep, a_slot, b_slot, psT, want_h=False):
        i = ctx.n % 2; ctx.n += 1
        x = ctx.x[i]; h = ctx.h[i]; hT = ctx.hT[i]; ss = ctx.ss[i]; rs = ctx.rs[i]
        S.dma(SP, lambda e: e.dma_start(out=x[:, :], in_=src_rows_ap), src_dep, [x])
        rstd_of(ss, rs, x[:, :], x, ctx.junk)
        S.op(DVE, lambda e: e.scalar_tensor_tensor(out=ctx.t[:, :], in0=x[:, :], scalar=rs[:, 0:1], in1=mod[:, a_slot, :],
                                                   op0=ALU.mult, op1=ALU.mult), [x, rs, (mod, a_slot)], [ctx.t])
        S.op(DVE, lambda e: e.tensor_tensor(out=h[:, :], in0=ctx.t[:, :], in1=mod[:, b_slot, :], op=ALU.add),
             [ctx.t, (mod, b_slot)], [h])
        pb = PSB[psT]
        for k in range(8):
            S.op(PE, lambda e, k=k: e.transpose(out=pb[:, k * 128:(k + 1) * 128], in_=h[:, k * 128:(k + 1) * 128],
                                                identity=identb[:, :]), [h, identb], [PS[psT]])
        S.op(ACT, lambda e: e.copy(out=hT[:, :, :], in_=cols(pb[:, 0:1024], 8)), [PS[psT]], [hT])
        return (hT, h, x) if want_h else hT

    xo_v = xo.ap().rearrange("(t p) c -> t p c", p=128)
    xc_v = xc.ap().rearrange("(t p) c -> t p c", p=128)

    m1 = A.mark()
    wq = A.alloc([128, 8, 512], BF16, "wq"); wk = A.alloc([128, 8, 512], BF16, "wk")
    wv = A.alloc([128, 8, 512], BF16, "wv"); wf = A.alloc([128, 8, 8], BF16, "wf")
    wba = A.alloc([128, 4, 1024], BF16, "wba")
    load_w(wk, win_cols(512, 1024)); load_w(wv, win_cols(1024, 1536)); load_w(wf, win_cols(1536, 1544))
    load_w(wq, win_cols(0, 512))
    load_w(wba, w_ba.ap().rearrange("(k p) c -> p k c", p=128))
    kT_all = A.alloc([128, 4, 4096], BF16, "kT_all", 32)
    V_all = A.alloc([128, 32, 8, 65], BF16, "V_all", 32)
    G_all = A.alloc([128, 32, 8], F32, "G_all", 32)
    Gend = A.alloc([128, 33, 8], F32, "Gend", 33)
    bfg = A.alloc([128, 8], F32, "bfg")
    S.dma(SP, lambda e: e.dma_start(out=bfg[:, :], in_=bfg_bc[:, :]), [], [bfg])
    S.op(DVE, lambda e: e.memset(V_all[:, :, :, 64:65], 1.0), [], [V_all])
    S.op(DVE, lambda e: e.memset(Gend[:, 0, :], 0.0), [], [(Gend, 0)])
    ctx = NormCtx()
    lf = [A.alloc([128, 8], F32, f"lf{i}") for i in range(2)]
    qT = [A.alloc([128, 4, 128], BF16, f"qT{i}") for i in range(2)]
    biasb = [A.alloc([128, 8, 32], F32, f"biasb{i}") for i in range(2)]
    PT = [A.alloc([128, 128], BF16, f"PT{i}") for i in range(8)]
    rden = [A.alloc([128, 8], F32, f"rden{i}") for i in range(2)]
    ao = [A.alloc([128, 512], BF16, f"ao{i}") for i in range(2)]
    aoT = [A.alloc([128, 4, 128], BF16, f"aoT{i}") for i in range(2)]
    yab = [A.alloc([128, 8, 128], BF16, f"yab{i}") for i in range(2)]
    ptc = [0]

    def fox_kvf(hT, gb, it):
        for pr in range(4):
            for k in range(8):
                S.op(PE, lambda e, pr=pr, k=k: e.matmul(PS[1][:, pr * 128:(pr + 1) * 128], lhsT=wk[:, k, pr * 128:(pr + 1) * 128],
                                                        rhs=hT[:, k, :], start=(k == 0), stop=(k == 7)), [wk, hT], [PS[1]])
        S.op(ACT, lambda e: e.copy(out=kT_all[:, :, gb * 128:(gb + 1) * 128], in_=cols(PS[1][:, 0:512], 4)),
             [PS[1]], [(kT_all, gb)])
        for k in range(8):
            S.op(PE, lambda e, k=k: e.matmul(PS[2][:, 0:512], lhsT=hT[:, k, :], rhs=wv[:, k, :],
                                             start=(k == 0), stop=(k == 7)), [wv, hT], [PS[2]])
        S.op(DVE, lambda e: e.tensor_copy(out=V_all[:, gb, :, 0:64], in_=cols(PS[2][:, 0:512], 8)), [PS[2]], [(V_all, gb)])
        for k in range(8):
            S.op(PE, lambda e, k=k: e.matmul(PS[4][:, 384:392], lhsT=hT[:, k, :], rhs=wf[:, k, :],
                                             start=(k == 0), stop=(k == 7)), [wf, hT], [(PS[4], 3)])
        l = lf[it % 2]
        S.op(DVE, lambda e: e.tensor_tensor(out=l[:, :], in0=PS[4][:, 384:392], in1=bfg[:, :], op=ALU.add), [(PS[4], 3), bfg], [l])
        S.op(ACT, lambda e: e.activation(out=l[:, :], in_=l[:, :], func=AF.Exp, scale=-1.0), [l], [l])
        S.op(ACT, lambda e: e.activation(out=l[:, :], in_=l[:, :], func=AF.Ln, bias=1.0), [l], [l])
        S.op(PE, lambda e: e.matmul(PS[4][:, 400:408], lhsT=trif[:, :], rhs=l[:, :], start=True, stop=True), [trif, l], [(PS[4], 3)])
        S.op(PE, lambda e: e.matmul(PS[4][:, 416:424], lhsT=onesf[:, :], rhs=l[:, :], start=True, stop=True), [onesf, l], [(PS[4], 3)])
        S.op(DVE, lambda e: e.tensor_tensor(out=G_all[:, gb, :], in0=PS[4][:, 400:408], in1=Gend[:, gb, :], op=ALU.add),
             [(PS[4], 3), (Gend, gb)], [(G_all, gb)])
        S.op(DVE, lambda e: e.tensor_tensor(out=Gend[:, gb + 1, :], in0=PS[4][:, 416:424], in1=Gend[:, gb, :], op=ALU.add),
             [(PS[4], 3), (Gend, gb)], [(Gend, gb + 1)])

    for it in range(NT):
        hT = norm_hT(ctx, xc_v[it], [], 1, 0, 0)
        fox_kvf(hT, it, it)
    S.op(DVE, lambda e: e.tensor_scalar(out=G_all[:, 0:16, :], in0=G_all[:, 0:16, :], scalar1=omf[:, 0:1], scalar2=None,
                                        op0=ALU.add), [(G_all, range(16)), omf], [(G_all, range(16))])
    S.op(DVE, lambda e: e.tensor_scalar(out=Gend[:, 16, :], in0=Gend[:, 16, :], scalar1=flag[:, 0:1], scalar2=None,
                                        op0=ALU.mult), [(Gend, 16), flag], [(Gend, 16)])

    for it in range(NT):
        gb = 16 + it
        hT = norm_hT(ctx, xo_v[it], [], 1, 0, 0)
        q = qT[it % 2]
        for pr in range(4):
            for k in range(8):
                S.op(PE, lambda e, pr=pr, k=k: e.matmul(PS[3][:, pr * 128:(pr + 1) * 128], lhsT=wq[:, k, pr * 128:(pr + 1) * 128],
                                                        rhs=hT[:, k, :], start=(k == 0), stop=(k == 7)), [wq, hT], [PS[3]])
        S.op(ACT, lambda e: e.copy(out=q[:, :, :], in_=cols(PS[3][:, 0:512], 4)), [PS[3]], [q])
        fox_kvf(hT, gb, it)
        bb = biasb[it % 2]
        nk = gb + 1
        for h in range(8):
            S.op(DVE, lambda e, h=h: e.tensor_scalar(out=bb[:, h, 0:nk], in0=G_all[:, 0:nk, h], scalar1=Gend[:, gb + 1, h:h + 1],
                                                     scalar2=None, op0=ALU.subtract),
                 [(G_all, range(nk)), (Gend, gb + 1)], [bb])
        items = [(h, kb) for h in range(8) for kb in range(nk)]
        chunks = []
        for h_ in range(8):
            hi = [(h_, kb_) for kb_ in range(nk)]
            chunks += [hi[i:i + 4] for i in range(0, nk, 4)]

        def emit_qk(ci):
            sb = PS[6 + ci % 2]
            for j, (h, kb) in enumerate(chunks[ci]):
                pr, off = h // 2, (h % 2) * 64
                S.op(PE, lambda e: e.matmul(sb[:, j * 128:(j + 1) * 128], lhsT=kT_all[off:off + 64, pr, kb * 128:(kb + 1) * 128],
                                            rhs=q[off:off + 64, pr, :], start=True, stop=True), [(kT_all, kb), q], [sb])
            for j, (h, kb) in enumerate(chunks[ci]):
                pt = PT[(ci % 2) * 4 + j]
                S.op(ACT, lambda e: e.activation(out=pt[:, :], in_=sb[:, j * 128:(j + 1) * 128], func=AF.Exp,
                                                 bias=bb[:, h, kb:kb + 1], scale=0.125), [sb, bb], [pt])
                if kb == gb:
                    S.op(DVE, lambda e: e.tensor_tensor(out=pt[:, :], in0=pt[:, :], in1=trib[:, :], op=ALU.mult), [pt, trib], [pt])

        def emit_pv(ci):
            for j, (h, kb) in enumerate(chunks[ci]):
                pt = PT[(ci % 2) * 4 + j]
                ob = PS[4] if h < 4 else PS[5]
                oc = (h % 4) * 65
                S.op(PE, lambda e: e.matmul(ob[:, oc:oc + 65], lhsT=pt[:, :], rhs=V_all[:, kb, h, :], start=(kb == 0), stop=(kb == gb)),
                     [pt, (V_all, kb)], [(ob, (0, 1, 2))])

        for ci in range(len(chunks)):
            emit_qk(ci)
            if ci > 0:
                emit_pv(ci - 1)
        emit_pv(len(chunks) - 1)
        rd = rden[it % 2]; a_ = ao[it % 2]; aT = aoT[it % 2]; yb = yab[it % 2]
        for hh in range(2):
            ob = PS[4 + hh]
            ov = ob[:, 0:260].rearrange("p (h c) -> p h c", h=4)
            S.op(DVE, lambda e, ov=ov, hh=hh: e.reciprocal(out=rd[:, hh * 4:(hh + 1) * 4], in_=ov[:, :, 64]), [(ob, (0, 1, 2))], [rd])
            for h4 in range(4):
                h = hh * 4 + h4
                S.op(DVE, lambda e, ov=ov, h=h, h4=h4: e.tensor_scalar(out=a_[:, h * 64:(h + 1) * 64], in0=ov[:, h4, 0:64],
                                                                      scalar1=rd[:, h:h + 1], scalar2=None, op0=ALU.mult),
                     [(ob, (0, 1, 2)), rd], [a_])
        for j in range(4):
            S.op(PE, lambda e, j=j: e.transpose(out=PSB[0][:, j * 128:(j + 1) * 128], in_=a_[:, j * 128:(j + 1) * 128],
                                                identity=identb[:, :]), [a_, identb], [PS[0]])
        S.op(ACT, lambda e: e.copy(out=aT[:, :, :], in_=cols(PSB[0][:, 0:512], 4)), [PS[0]], [aT])
        for c in range(8):
            pbk = PS[1] if c < 4 else PS[2]
            for j in range(4):
                S.op(PE, lambda e, c=c, j=j, pbk=pbk: e.matmul(pbk[:, (c % 4) * 128:(c % 4 + 1) * 128],
                                                              lhsT=wba[:, j, c * 128:(c + 1) * 128], rhs=aT[:, j, :],
                                                              start=(j == 0), stop=(j == 3)), [wba, aT], [pbk])
        S.op(ACT, lambda e: e.copy(out=yb[:, 0:4, :], in_=cols(PS[1][:, 0:512], 4)), [PS[1]], [yb])
        S.op(ACT, lambda e: e.copy(out=yb[:, 4:8, :], in_=cols(PS[2][:, 0:512], 4)), [PS[2]], [yb])
        S.dma(SP, lambda e, it=it: e.dma_start(out=YA[it].rearrange("p (c t) -> p c t", c=8), in_=yb[:, :, :]), [yb], [(YAb, it)])
    A.release(m1)
    S.barrier()

    YB = nc.dram_tensor("yb_scr", [16, 128, 1024], BF16)
    YBb = Buf(YB, 16)
    m2 = A.mark()
    wrq = A.alloc([128, 8, 512], BF16, "wrq"); wrk = A.alloc([128, 8, 512], BF16, "wrk")
    wrv = A.alloc([128, 8, 1024], BF16, "wrv"); wrg = A.alloc([128, 8, 1024], BF16, "wrg")
    wgb = A.alloc([128, 8, 1024], BF16, "wgb"); wbb = A.alloc([128, 8, 1024], BF16, "wbb")
    load_w(wrk, win_cols(2056, 2568)); load_w(wrv, win_cols(2568, 3592)); load_w(wrq, win_cols(1544, 2056))
    load_w(wrg, win_cols(3592, 4616)); load_w(wgb, win_cols(5640, 6664))
    load_w(wbb, w_bb.ap().rearrange("(k p) c -> p k c", p=128))
    gng = A.alloc([128, 8], F32, "gng"); cdt = A.alloc([128, 4, 128], F32, "cdt")
    S.dma(SP, lambda e: e.dma_start(out=gng[:, :], in_=gng_d[:, :]), [], [gng])
    S.dma(SP, lambda e: e.dma_start(out=cdt[:, :, :], in_=cdt_d.ap().rearrange("p (m c) -> p m c", m=4)), [], [cdt])
    for h in range(8):
        S.op(DVE, lambda e, h=h: e.tensor_scalar(out=wbb[:, h, :], in0=wbb[:, h, :], scalar1=gng[:, h:h + 1], scalar2=None,
                                                 op0=ALU.mult), [wbb, gng], [wbb])
    St = A.alloc([128, 4, 128], F32, "St"); Sbf = A.alloc([128, 4, 128], BF16, "Sbf")
    S.op(DVE, lambda e: e.memset(St[:, :, :], 0.0), [], [St])
    S.op(DVE, lambda e: e.memset(Sbf[:, :, :], 0.0), [], [Sbf])
    ctx = NormCtx()
    tabs = [A.alloc([128, 256], F32, f"tab{i}") for i in range(4)]
    rt = [A.alloc([128, 8, 32], F32, f"rt{i}") for i in range(2)]
    qt = A.alloc([128, 8, 64], BF16, "qt"); kt = A.alloc([128, 8, 64], BF16, "kt")
    qtT = A.alloc([128, 4, 128], BF16, "qtT"); ktT = A.alloc([128, 4, 128], BF16, "ktT")
    vb = A.alloc([128, 8, 128], BF16, "vb")
    sig = A.alloc([128, 4, 128], F32, "sig")
    srg = A.alloc([128, 8, 128], BF16, "srg"); sgb = A.alloc([128, 8, 128], BF16, "sgb")
    msc = [A.alloc([128, 128], BF16, f"msc{i}") for i in range(4)]
    osb = A.alloc([128, 8, 128], F32, "osb"); osq = A.alloc([128, 8, 128], F32, "osq")
    mean = A.alloc([128, 8, 128], F32, "mean"); var = A.alloc([128, 8, 128], F32, "var")
    og = A.alloc([128, 8, 128], BF16, "og"); ybg = [A.alloc([128, 8, 128], BF16, f"ybg{i}") for i in range(2)]
    onesn = A.alloc([128, 128], F32, "onesn")
    S.op(DVE, lambda e: e.memset(onesn[:, :], 1.0 / 128), [], [onesn])

    def rotary(psb, tc, ts, dst):
        xv = cols(psb[:, 0:512], 8)
        x1, x2 = xv[:, :, 0:32], xv[:, :, 32:64]
        C = cols(tc[:, :], 8); Sn = cols(ts[:, :], 8)
        S.op(DVE, lambda e: e.tensor_tensor(out=rt[0][:, :, :], in0=x1, in1=C, op=ALU.mult), [psb, tc], [rt[0]])
        S.op(DVE, lambda e: e.tensor_tensor(out=rt[1][:, :, :], in0=x2, in1=Sn, op=ALU.mult), [psb, ts], [rt[1]])
        S.op(DVE, lambda e: e.tensor_tensor(out=dst[:, :, 0:32], in0=rt[0][:, :, :], in1=rt[1][:, :, :], op=ALU.subtract),
             [rt[0], rt[1]], [dst])
        S.op(DVE, lambda e: e.tensor_tensor(out=rt[0][:, :, :], in0=x1, in1=Sn, op=ALU.mult), [psb, ts], [rt[0]])
        S.op(DVE, lambda e: e.tensor_tensor(out=rt[1][:, :, :], in0=x2, in1=C, op=ALU.mult), [psb, tc], [rt[1]])
        S.op(DVE, lambda e: e.tensor_tensor(out=dst[:, :, 32:64], in0=rt[0][:, :, :], in1=rt[1][:, :, :], op=ALU.add),
             [rt[0], rt[1]], [dst])

    def ret_kv(hT, it, kc_name, ks_name):
        S.dma(SP, lambda e: e.dma_start(out=tabs[0][:, :], in_=rot[kc_name][it * 128:(it + 1) * 128, :]), [], [tabs[0]])
        S.dma(SP, lambda e: e.dma_start(out=tabs[1][:, :], in_=rot[ks_name][it * 128:(it + 1) * 128, :]), [], [tabs[1]])
        for k in range(8):
            S.op(PE, lambda e, k=k: e.matmul(PS[2][:, 0:512], lhsT=hT[:, k, :], rhs=wrk[:, k, :], start=(k == 0), stop=(k == 7)),
                 [hT, wrk], [PS[2]])
        for hh in range(2):
            for k in range(8):
                S.op(PE, lambda e, k=k, hh=hh: e.matmul(PS[3 + hh][:, 0:512], lhsT=hT[:, k, :], rhs=wrv[:, k, hh * 512:(hh + 1) * 512],
                                                        start=(k == 0), stop=(k == 7)), [hT, wrv], [PS[3 + hh]])
        rotary(PS[2], tabs[0], tabs[1], kt)
        for hh in range(2):
            S.op(ACT, lambda e, hh=hh: e.copy(out=vb[:, hh * 4:(hh + 1) * 4, :], in_=cols(PS[3 + hh][:, 0:512], 4)), [PS[3 + hh]], [vb])

    def state_update():
        for h in range(8):
            pr = h // 2
            pb_ = PS[6 + h // 4]
            S.op(PE, lambda e, h=h, pr=pr, pb_=pb_: e.matmul(pb_[:, (h % 4) * 128:(h % 4 + 1) * 128],
                                                            lhsT=kt[:, :, :].rearrange("p h c -> p (h c)")[:, pr * 128:(pr + 1) * 128],
                                                            rhs=vb[:, h, :], start=True, stop=True), [kt, vb], [pb_])
        for bi in range(2):
            for par in range(2):
                src = PS[6 + bi][par * 64:(par + 1) * 64, 0:512].rearrange("p (m two c) -> p m two c", two=2, c=128)[:, :, par, :]
                dst = St[par * 64:(par + 1) * 64, 2 * bi:2 * bi + 2, :]
                S.op(DVE, lambda e, src=src, dst=dst: e.tensor_tensor(out=dst, in0=src, in1=dst, op=ALU.add), [PS[6 + bi], St], [St])
        S.op(DVE, lambda e: e.tensor_tensor(out=St[:, :, :], in0=St[:, :, :], in1=cdt[:, :, :], op=ALU.mult), [St, cdt], [St])

    for it in range(NT):
        hT = norm_hT(ctx, xc_v[it], [], 1, 0, 0)
        ret_kv(hT, it, "rkc_c", "rkc_s")
        state_update()
    S.op(DVE, lambda e: e.tensor_scalar(out=St[:, :, :], in0=St[:, :, :], scalar1=flag[:, 0:1], scalar2=None, op0=ALU.mult),
         [St, flag], [St])
    S.op(DVE, lambda e: e.tensor_copy(out=Sbf[:, :, :], in_=St[:, :, :]), [St], [Sbf])

    for it in range(NT):
        hT = norm_hT(ctx, xo_v[it], [], 1, 0, 0)
        S.dma(SP, lambda e, it=it: e.dma_start(out=tabs[2][:, :], in_=rot["rqo_c"][it * 128:(it + 1) * 128, :]), [], [tabs[2]])
        S.dma(SP, lambda e, it=it: e.dma_start(out=tabs[3][:, :], in_=rot["rqo_s"][it * 128:(it + 1) * 128, :]), [], [tabs[3]])
        for k in range(8):
            S.op(PE, lambda e, k=k: e.matmul(PS[1][:, 0:512], lhsT=hT[:, k, :], rhs=wrq[:, k, :], start=(k == 0), stop=(k == 7)),
                 [hT, wrq], [PS[1]])
        ret_kv(hT, it, "rko_c", "rko_s")
        rotary(PS[1], tabs[2], tabs[3], qt)
        for (wsrc, dst, is_silu) in ((wrg, srg, True), (wgb, sgb, False)):
            for hh in range(2):
                for c4 in range(4):
                    c = hh * 4 + c4
                    for k in range(8):
                        S.op(PE, lambda e, c=c, c4=c4, k=k, wsrc=wsrc: e.matmul(PS[5][:, c4 * 128:(c4 + 1) * 128],
                                                                               lhsT=wsrc[:, k, c * 128:(c + 1) * 128], rhs=hT[:, k, :],
                                                                               start=(k == 0), stop=(k == 7)), [wsrc, hT], [PS[5]])
                if is_silu:
                    S.op(ACT, lambda e: e.activation(out=sig[:, :, :], in_=cols(PS[5][:, 0:512], 4), func=AF.Sigmoid), [PS[5]], [sig])
                    S.op(DVE, lambda e, hh=hh, dst=dst: e.tensor_tensor(out=dst[:, hh * 4:(hh + 1) * 4, :], in0=cols(PS[5][:, 0:512], 4),
                                                                       in1=sig[:, :, :], op=ALU.mult), [PS[5], sig], [dst])
                else:
                    S.op(ACT, lambda e, hh=hh, dst=dst: e.activation(out=dst[:, hh * 4:(hh + 1) * 4, :], in_=cols(PS[5][:, 0:512], 4),
                                                                    func=AF.Sigmoid), [PS[5]], [dst])
        qf = qt[:, :, :].rearrange("p h c -> p (h c)"); kf = kt[:, :, :].rearrange("p h c -> p (h c)")
        for j in range(4):
            S.op(PE, lambda e, j=j: e.transpose(out=PSB[0][:, j * 128:(j + 1) * 128], in_=qf[:, j * 128:(j + 1) * 128],
                                                identity=identb[:, :]), [qt, identb], [PS[0]])
        for j in range(4):
            S.op(PE, lambda e, j=j: e.transpose(out=PSB[0][:, 512 + j * 128:512 + (j + 1) * 128], in_=kf[:, j * 128:(j + 1) * 128],
                                                identity=identb[:, :]), [kt, identb], [PS[0]])
        S.op(ACT, lambda e: e.copy(out=qtT[:, :, :], in_=cols(PSB[0][:, 0:512], 4)), [PS[0]], [qtT])
        S.op(ACT, lambda e: e.copy(out=ktT[:, :, :], in_=cols(PSB[0][:, 512:1024], 4)), [PS[0]], [ktT])
        for h in range(8):
            pr, off = h // 2, (h % 2) * 64
            mb = msc[h % 4]
            ob = PS[6 + h // 4]; oq = h % 4
            S.op(PE, lambda e, h=h, pr=pr, off=off: e.matmul(PS[5][:, (h % 4) * 128:(h % 4 + 1) * 128], lhsT=ktT[off:off + 64, pr, :],
                                                            rhs=qtT[off:off + 64, pr, :], start=True, stop=True),
                 [ktT, qtT], [(PS[5], h % 4)])
            S.op(DVE, lambda e, h=h, mb=mb: e.tensor_tensor(out=mb[:, :], in0=PS[5][:, (h % 4) * 128:(h % 4 + 1) * 128], in1=trib[:, :],
                                                           op=ALU.mult), [(PS[5], h % 4), trib], [mb])
            S.op(PE, lambda e, h=h, mb=mb, ob=ob, oq=oq: e.matmul(ob[:, oq * 128:(oq + 1) * 128], lhsT=vb[:, h, :], rhs=mb[:, :],
                                                                 start=True, stop=False), [vb, mb], [(ob, oq)])
            S.op(PE, lambda e, pr=pr, off=off, ob=ob, oq=oq: e.matmul(ob[:, oq * 128:(oq + 1) * 128], lhsT=Sbf[off:off + 64, pr, :],
                                                                     rhs=qtT[off:off + 64, pr, :], start=False, stop=True),
                 [Sbf, qtT], [(ob, oq)])
        for hh in range(2):
            S.op(ACT, lambda e, hh=hh: e.copy(out=osb[:, hh * 4:(hh + 1) * 4, :], in_=cols(PS[6 + hh][:, 0:512], 4)), [PS[6 + hh]], [osb])
            S.op(ACT, lambda e, hh=hh: e.activation(out=osq[:, hh * 4:(hh + 1) * 4, :], in_=cols(PS[6 + hh][:, 0:512], 4), func=AF.Square),
                 [PS[6 + hh]], [osq])
        state_update()
        S.op(DVE, lambda e: e.tensor_copy(out=Sbf[:, :, :], in_=St[:, :, :]), [St], [Sbf])
        for hh in range(2):
            S.op(PE, lambda e, hh=hh: e.matmul(PS[3 + hh][:, 0:512], lhsT=onesn[:, :],
                                               rhs=osb[:, hh * 4:(hh + 1) * 4, :].rearrange("p h c -> p (h c)"), start=True, stop=True),
                 [onesn, osb], [PS[3 + hh]])
            S.op(PE, lambda e, hh=hh: e.matmul(PS[1 + hh][:, 0:512], lhsT=onesn[:, :],
                                               rhs=osq[:, hh * 4:(hh + 1) * 4, :].rearrange("p h c -> p (h c)"), start=True, stop=True),
                 [onesn, osq], [PS[1 + hh]])
        for hh in range(2):
            sl = slice(hh * 4, (hh + 1) * 4)
            S.op(ACT, lambda e, hh=hh, sl=sl: e.copy(out=mean[:, sl, :], in_=cols(PS[3 + hh][:, 0:512], 4)), [PS[3 + hh]], [mean])
            S.op(DVE, lambda e, sl=sl: e.tensor_tensor(out=var[:, sl, :], in0=mean[:, sl, :], in1=mean[:, sl, :], op=ALU.mult), [mean], [var])
            S.op(DVE, lambda e, hh=hh, sl=sl: e.tensor_tensor(out=var[:, sl, :], in0=cols(PS[1 + hh][:, 0:512], 4), in1=var[:, sl, :],
                                                             op=ALU.subtract), [PS[1 + hh], var], [var])
        S.op(DVE, lambda e: e.tensor_scalar(out=var[:, :, :], in0=var[:, :, :], scalar1=0.0, scalar2=1e-6, op0=ALU.max, op1=ALU.add),
             [var], [var])
        S.op(ACT, lambda e: e.activation(out=var[:, :, :], in_=var[:, :, :], func=AF.Ln), [var], [var])
        S.op(ACT, lambda e: e.activation(out=var[:, :, :], in_=var[:, :, :], func=AF.Exp, scale=-0.5), [var], [var])
        S.op(DVE, lambda e: e.tensor_tensor(out=osb[:, :, :], in0=osb[:, :, :], in1=mean[:, :, :], op=ALU.subtract), [osb, mean], [osb])
        S.op(DVE, lambda e: e.tensor_tensor(out=osb[:, :, :], in0=osb[:, :, :], in1=var[:, :, :], op=ALU.mult), [osb, var], [osb])
        S.op(DVE, lambda e: e.tensor_tensor(out=og[:, :, :], in0=osb[:, :, :], in1=srg[:, :, :], op=ALU.mult), [osb, srg], [og])
        yb_ = ybg[it % 2]
        for c in range(8):
            pbk = PS[3 + c // 4]
            for h in range(8):
                S.op(PE, lambda e, c=c, h=h, pbk=pbk: e.matmul(pbk[:, (c % 4) * 128:(c % 4 + 1) * 128], lhsT=wbb[:, h, c * 128:(c + 1) * 128],
                                                              rhs=og[:, h, :], start=(h == 0), stop=(h == 7)), [wbb, og], [pbk])
        for hh in range(2):
            S.op(DVE, lambda e, hh=hh, yb_=yb_: e.tensor_tensor(out=yb_[:, hh * 4:(hh + 1) * 4, :], in0=cols(PS[3 + hh][:, 0:512], 4),
                                                               in1=sgb[:, hh * 4:(hh + 1) * 4, :], op=ALU.mult), [PS[3 + hh], sgb], [yb_])
        S.dma(SP, lambda e, it=it, yb_=yb_: e.dma_start(out=YB[it].rearrange("p (c t) -> p c t", c=8), in_=yb_[:, :, :]), [yb_], [(YBb, it)])
    A.release(m2)
    S.barrier()

    m3 = A.mark()
    wga = A.alloc([128, 8, 1024], BF16, "wga"); wout = A.alloc([128, 8, 1024], BF16, "wout")
    load_w(wga, win_cols(4616, 5640)); load_w(wout, w_out.ap().rearrange("(k p) c -> p k c", p=128))
    ctx = NormCtx()
    yat = [A.alloc([128, 8, 128], BF16, f"yat{i}") for i in range(2)]
    ybt = [A.alloc([128, 8, 128], BF16, f"ybt{i}") for i in range(2)]
    sga = A.alloc([128, 8, 128], F32, "sga")
    mrg = [A.alloc([128, 8, 128], BF16, f"mrg{i}") for i in range(2)]
    x1t = [A.alloc([128, 1024], F32, f"x1t{i}") for i in range(2)]
    for it in range(NT):
        hT, _h, xt = norm_hT(ctx, xo_v[it], [], 1, 0, 0, want_h=True)
        ya_ = yat[it % 2]; yb_ = ybt[it % 2]; mg = mrg[it % 2]; xo_ = x1t[it % 2]
        S.dma(SP, lambda e, it=it, ya_=ya_: e.dma_start(out=ya_[:, :, :], in_=YA[it].rearrange("p (c t) -> p c t", c=8)), [(YAb, it)], [ya_])
        S.dma(SP, lambda e, it=it, yb_=yb_: e.dma_start(out=yb_[:, :, :], in_=YB[it].rearrange("p (c t) -> p c t", c=8)), [(YBb, it)], [yb_])
        for hh in range(2):
            for c4 in range(4):
                c = hh * 4 + c4
                for k in range(8):
                    S.op(PE, lambda e, c=c, c4=c4, k=k, hh=hh: e.matmul(PS[1 + hh][:, c4 * 128:(c4 + 1) * 128],
                                                                       lhsT=wga[:, k, c * 128:(c + 1) * 128], rhs=hT[:, k, :],
                                                                       start=(k == 0), stop=(k == 7)), [wga, hT], [PS[1 + hh]])
            S.op(ACT, lambda e, hh=hh: e.activation(out=sga[:, hh * 4:(hh + 1) * 4, :], in_=cols(PS[1 + hh][:, 0:512], 4), func=AF.Sigmoid),
                 [PS[1 + hh]], [sga])
        S.op(DVE, lambda e, ya_=ya_: e.tensor_tensor(out=sga[:, :, :], in0=sga[:, :, :], in1=ya_[:, :, :], op=ALU.mult), [sga, ya_], [sga])
        S.op(DVE, lambda e, yb_=yb_, mg=mg: e.tensor_tensor(out=mg[:, :, :], in0=sga[:, :, :], in1=yb_[:, :, :], op=ALU.add), [sga, yb_], [mg])
        for hh in range(2):
            for c in range(8):
                S.op(PE, lambda e, c=c, hh=hh, mg=mg: e.matmul(PS[3 + hh][:, 0:512], lhsT=mg[:, c, :], rhs=wout[:, c, hh * 512:(hh + 1) * 512],
                                                              start=(c == 0), stop=(c == 7)), [mg, wout], [PS[3 + hh]])
            S.op(DVE, lambda e, hh=hh, xo_=xo_: e.tensor_tensor(out=xo_[:, hh * 512:(hh + 1) * 512], in0=PS[3 + hh][:, 0:512],
                                                               in1=mod[:, 2, hh * 512:(hh + 1) * 512], op=ALU.mult),
                 [PS[3 + hh], (mod, 2)], [xo_])
        S.op(DVE, lambda e, xo_=xo_, xt=xt: e.tensor_tensor(out=xo_[:, :], in0=xo_[:, :], in1=xt[:, :], op=ALU.add), [xo_, xt], [xo_])
        S.dma(SP, lambda e, it=it, xo_=xo_: e.dma_start(out=X1[it * 128:(it + 1) * 128, :], in_=xo_[:, :]), [xo_], [(X1b, it)])
    A.release(m3)
    S.barrier()

    CAP = 256; RND = CAP // 8; NSB = CAP // 128; NACC = 4
    H2 = nc.dram_tensor("h2_scr", [2176, 1024], BF16)
    ACCd = [nc.dram_tensor(f"acc_scr{a}", [2176, 1024], F32) for a in range(NACC)]
    H2b = Buf(H2, 17); ACCb = [Buf(ACCd[a], 17) for a in range(NACC)]
    m4 = A.mark()
    h2T = A.alloc([128, 8, 2048], BF16, "h2T", 16)
    Wt = A.alloc([128, 16, 256], F32, "Wt", 16)
    idxT = A.alloc([128, 2, NSB, 128], I32, "idxT"); wT = A.alloc([128, 2, NSB, 128], F32, "wT")
    iot = A.alloc([128, 16], I32, "iot")
    S.dma(SP, lambda e: e.dma_start(out=iot[:, :], in_=iota_d[:, :]), [], [iot])
    mr = A.mark()
    wr = A.alloc([128, 8, 256], BF16, "wr"); rb = A.alloc([128, 256], F32, "rb")
    load_w(wr, w_router.ap().rearrange("(k p) c -> p k c", p=128))
    S.dma(SP, lambda e: e.dma_start(out=rb[:, :], in_=rbias_bc[:, :]), [], [rb])
    zf = A.alloc([128, 1024], F32, "zf"); zb = A.alloc([128, 1024], BF16, "zb")
    S.op(DVE, lambda e: e.memset(zf[:, :], 0.0), [], [zf])
    S.op(DVE, lambda e: e.memset(zb[:, :], 0.0), [], [zb])
    S.dma(SP, lambda e: e.dma_start(out=H2[2048:2176, :], in_=zb[:, :]), [zb], [(H2b, 16)])
    for a in range(NACC):
        for r_ in range(17):
            S.dma(SP, lambda e: e.dma_start(out=ACCd[a][r_ * 128:(r_ + 1) * 128, :], in_=zf[:, :]), [zf], [(ACCb[a], r_)])
    ctx = NormCtx()
    sc = A.alloc([128, 256], F32, "sc"); sel = A.alloc([128, 256], F32, "sel"); selm = A.alloc([128, 256], F32, "selm")
    t8 = A.alloc([128, 8, 8], F32, "t8"); gs = A.alloc([128, 8], F32, "gs"); gt8 = A.alloc([128, 8], F32, "gt8")
    gm = A.alloc([128, 8], F32, "gm"); gp = A.alloc([128, 8], F32, "gp"); e8 = A.alloc([128, 8], F32, "e8")
    em = A.alloc([128, 256], F32, "em"); dn = A.alloc([128, 1], F32, "dn")
    for it in range(NT):
        hT, hh_, _x = norm_hT(ctx, X1[it * 128:(it + 1) * 128, :], [(X1b, it)], 4, 3, 0, want_h=True)
        S.dma(SP, lambda e: e.dma_start(out=H2[it * 128:(it + 1) * 128, :], in_=hh_[:, :]), [hh_], [(H2b, it)])
        S.op(DVE, lambda e: e.tensor_copy(out=h2T[:, :, it * 128:(it + 1) * 128], in_=hT[:, :, :]), [hT], [(h2T, it)])
        for k in range(8):
            S.op(PE, lambda e: e.matmul(PS[1][:, 0:256], lhsT=hT[:, k, :], rhs=wr[:, k, :], start=(k == 0), stop=(k == 7)),
                 [hT, wr], [PS[1]])
        S.op(ACT, lambda e: e.activation(out=sc[:, :], in_=PS[1][:, 0:256], func=AF.Sigmoid), [PS[1]], [sc])
        S.op(DVE, lambda e: e.tensor_tensor(out=sel[:, :], in0=sc[:, :], in1=rb[:, :], op=ALU.add), [sc, rb], [sel])
        for g in range(8):
            S.op(DVE, lambda e: e.max(out=t8[:, g, :], in_=sel[:, g * 32:(g + 1) * 32]), [sel], [t8])
        S.op(DVE, lambda e: e.tensor_tensor(out=gs[:, :], in0=t8[:, :, 0], in1=t8[:, :, 1], op=ALU.add), [t8], [gs])
        S.op(DVE, lambda e: e.max(out=gt8[:, :], in_=gs[:, :]), [gs], [gt8])
        S.op(DVE, lambda e: e.tensor_scalar(out=gm[:, :], in0=gs[:, :], scalar1=gt8[:, 3:4], scalar2=None, op0=ALU.is_ge), [gs, gt8], [gm])
        S.op(DVE, lambda e: e.tensor_scalar(out=gp[:, :], in0=gm[:, :], scalar1=-1.0, scalar2=1.0e4, op0=ALU.add, op1=ALU.mult), [gm], [gp])
        for g in range(8):
            S.op(DVE, lambda e: e.tensor_scalar(out=selm[:, g * 32:(g + 1) * 32], in0=sel[:, g * 32:(g + 1) * 32],
                                                scalar1=gm[:, g:g + 1], scalar2=gp[:, g:g + 1], op0=ALU.mult, op1=ALU.add),
                 [sel, gm, gp], [selm])
        S.op(DVE, lambda e: e.max(out=e8[:, :], in_=selm[:, :]), [selm], [e8])
        S.op(DVE, lambda e: e.tensor_scalar(out=em[:, :], in0=selm[:, :], scalar1=e8[:, 7:8], scalar2=None, op0=ALU.is_ge), [selm, e8], [em])
        S.op(DVE, lambda e: e.tensor_tensor(out=em[:, :], in0=em[:, :], in1=sc[:, :], op=ALU.mult), [em, sc], [em])
        S.op(DVE, lambda e: e.tensor_reduce(out=dn[:, :], in_=em[:, :], axis=AX.X, op=ALU.add), [em], [dn])
        S.op(DVE, lambda e: e.reciprocal(out=dn[:, :], in_=dn[:, :]), [dn], [dn])
        S.op(DVE, lambda e: e.tensor_scalar(out=Wt[:, it, :], in0=em[:, :], scalar1=dn[:, 0:1], scalar2=2.5,
                                            op0=ALU.mult, op1=ALU.mult), [em, dn], [(Wt, it)])
    A.release(mr)
    S.barrier()
    mx = A.mark()
    WT = A.alloc([128, 2, 2048], F32, "WT", 2)
    vals = A.alloc([128, 2, CAP], F32, "vals", 2); idxu = A.alloc([128, 2, CAP], U32, "idxu", 2)
    idxf = A.alloc([128, 2, CAP], F32, "idxf", 2); vld = A.alloc([128, 2, CAP], F32, "vld", 2)
    for hf in range(2):
        for g4 in range(4):
            pb_ = PS[(hf * 4 + g4) % 4]
            for j in range(4):
                it = g4 * 4 + j
                S.op(PE, lambda e: e.matmul(pb_[:, j * 128:(j + 1) * 128], lhsT=Wt[:, it, hf * 128:(hf + 1) * 128], rhs=identf[:, :],
                                            start=True, stop=True), [(Wt, it), identf], [pb_])
            S.op(ACT, lambda e: e.copy(out=WT[:, hf, g4 * 512:(g4 + 1) * 512], in_=pb_[:, 0:512]), [pb_], [(WT, hf)])
    for r_ in range(RND):
        for hf in range(2):
            v8 = vals[:, hf, r_ * 8:(r_ + 1) * 8]
            S.op(DVE, lambda e: e.max(out=v8, in_=WT[:, hf, :]), [(WT, hf)], [(vals, hf)])
            S.op(DVE, lambda e: e.max_index(out=idxu[:, hf, r_ * 8:(r_ + 1) * 8], in_max=v8, in_values=WT[:, hf, :]),
                 [(WT, hf), (vals, hf)], [(idxu, hf)])
            S.op(DVE, lambda e: e.match_replace(out=WT[:, hf, :], in_to_replace=v8, in_values=WT[:, hf, :], imm_value=0.0),
                 [(WT, hf), (vals, hf)], [(WT, hf)])
    S.op(DVE, lambda e: e.tensor_copy(out=idxf[:, :, :], in_=idxu[:, :, :]), [idxu], [idxf])
    S.op(DVE, lambda e: e.tensor_single_scalar(out=vld[:, :, :], in_=vals[:, :, :], scalar=0.0, op=ALU.is_gt), [vals], [vld])
    dmy = A.alloc([128, 2, CAP], F32, "dmy")
    S.dma(SP, lambda e: e.dma_start(out=dmy[:, :, :], in_=dummy_d.ap().rearrange("p (a c) -> p a c", a=2)), [], [dmy])
    S.op(DVE, lambda e: e.tensor_tensor(out=idxf[:, :, :], in0=idxf[:, :, :], in1=dmy[:, :, :], op=ALU.subtract), [idxf, dmy], [idxf])
    S.op(DVE, lambda e: e.tensor_tensor(out=idxf[:, :, :], in0=idxf[:, :, :], in1=vld[:, :, :], op=ALU.mult), [idxf, vld], [idxf])
    S.op(DVE, lambda e: e.tensor_tensor(out=idxf[:, :, :], in0=idxf[:, :, :], in1=dmy[:, :, :], op=ALU.add), [idxf, dmy], [idxf])
    S.op(DVE, lambda e: e.tensor_scalar(out=idxf[:, :, :], in0=idxf[:, :, :], scalar1=0.25, scalar2=None, op0=ALU.add), [idxf], [idxf])
    for hf in range(2):
        for sb_ in range(NSB):
            q_ = hf * NSB + sb_
            S.op(PE, lambda e: e.matmul(PS[4][:, q_ * 128:(q_ + 1) * 128], lhsT=idxf[:, hf, sb_ * 128:(sb_ + 1) * 128], rhs=identf[:, :],
                                        start=True, stop=True), [idxf, identf], [PS[4]])
            S.op(PE, lambda e: e.matmul(PS[5][:, q_ * 128:(q_ + 1) * 128], lhsT=vals[:, hf, sb_ * 128:(sb_ + 1) * 128], rhs=identf[:, :],
                                        start=True, stop=True), [vals, identf], [PS[5]])
    S.op(DVE, lambda e: e.tensor_copy(out=idxT[:, :, :, :].rearrange("p a b c -> p (a b c)"), in_=PS[4][:, 0:2 * NSB * 128]), [PS[4]], [idxT])
    S.op(ACT, lambda e: e.copy(out=wT[:, :, :, :].rearrange("p a b c -> p (a b c)"), in_=PS[5][:, 0:2 * NSB * 128]), [PS[5]], [wT])
    A.release(mx)
    S.barrier()
    NWS = 3
    _bc = {}

    def bc_reg(e):
        if "r" not in _bc:
            r_ = e.alloc_register("bcreg")
            e.reg_mov(r_, 2175)
            _bc["r"] = r_
        return _bc["r"]

    wgu = [A.alloc([128, 8, 512], BF16, f"wgu{i}") for i in range(NWS)]
    wdn = [A.alloc([128, 2, 1024], BF16, f"wdn{i}") for i in range(NWS)]
    Xg = [A.alloc([128, 1024], BF16, f"Xg{i}") for i in range(4)]
    XT = [A.alloc([128, 8, 128], BF16, f"XT{i}") for i in range(3)]
    sgm = [A.alloc([128, 256], F32, f"sgm{i}") for i in range(2)]
    tg = [A.alloc([128, 256], F32, f"tg{i}") for i in range(2)]
    Ab = [A.alloc([128, 256], BF16, f"Ab{i}") for i in range(2)]
    ATb = [A.alloc([128, 2, 128], BF16, f"ATb{i}") for i in range(2)]
    Yb = [A.alloc([128, 1024], F32, f"Yb{i}") for i in range(2)]
    blocks = []
    for ex in range(256):
        hf, el = ex // 128, ex % 128
        for sb_ in range(NSB):
            blocks.append((ex, idxT[:, hf, sb_, el:el + 1], wT[:, hf, sb_, el:el + 1], None))
    for it in range(NT):
        blocks.append((256, iot[:, it:it + 1], onesf[:, 0:1], it))
    NB = len(blocks)

    stg_g = [A.alloc([128, 8, 256], F32, f"stg_g{i}") for i in range(2)]
    stg_u = [A.alloc([128, 8, 256], F32, f"stg_u{i}") for i in range(2)]
    stg_dn = [A.alloc([128, 2, 1024], F32, f"stg_dn{i}") for i in range(2)]

    def stage_expert(ex):
        st_ = ex % 2
        if ex < 256:
            g_ap = w_eg.ap()[ex].rearrange("(p k) c -> p k c", k=8)
            u_ap = w_eu.ap()[ex].rearrange("(p k) c -> p k c", k=8)
            d_ap = w_ed.ap()[ex].rearrange("(p k) c -> p k c", k=2)
        else:
            g_ap = w_sg.ap().rearrange("(k p) c -> p k c", p=128)
            u_ap = w_su.ap().rearrange("(k p) c -> p k c", p=128)
            d_ap = w_sd.ap().rearrange("(k p) c -> p k c", p=128)
        S.dma(SP, lambda e: e.dma_start(out=stg_g[st_][:, :, :], in_=g_ap), [], [stg_g[st_]])
        S.dma(SP, lambda e: e.dma_start(out=stg_u[st_][:, :, :], in_=u_ap), [], [stg_u[st_]])
        S.dma(SP, lambda e: e.dma_start(out=stg_dn[st_][:, :, :], in_=d_ap), [], [stg_dn[st_]])

    def cast_expert(ex):
        st_ = ex % 2; sl = ex % NWS
        S.op(ACT, lambda e: e.copy(out=wgu[sl][:, :, 0:256], in_=stg_g[st_][:, :, :]), [stg_g[st_]], [wgu[sl]])
        S.op(ACT, lambda e: e.copy(out=wgu[sl][:, :, 256:512], in_=stg_u[st_][:, :, :]), [stg_u[st_]], [wgu[sl]])
        S.op(DVE, lambda e: e.tensor_copy(out=wdn[sl][:, :, :], in_=stg_dn[st_][:, :, :]), [stg_dn[st_]], [wdn[sl]])

    def st_gather(b):
        ex, ic, wc, sh = blocks[b]
        if sh is None:
            S.dma(POOL, lambda e: e.indirect_dma_start(out=Xg[b % 4][:, :], out_offset=None, in_=H2[:, :],
                                                       in_offset=bass.IndirectOffsetOnAxis(ap=ic, axis=0),
                                                       bounds_check=bc_reg(e), oob_is_err=False), [idxT, H2b], [Xg[b % 4]])

    def st_T(b):
        ex, ic, wc, sh = blocks[b]
        if sh is not None:
            return
        pT = 0 if b % 2 == 0 else 3
        for k in range(8):
            S.op(PE, lambda e: e.transpose(out=PSB[pT][:, k * 128:(k + 1) * 128],
                                           in_=Xg[b % 4][:, :].rearrange("s (p k) -> s k p", k=8)[:, k, :],
                                           identity=identb[:, :]), [Xg[b % 4], identb], [PS[pT]])
        S.op(ACT, lambda e: e.copy(out=XT[b % 3][:, :, :], in_=cols(PSB[pT][:, 0:1024], 8)), [PS[pT]], [XT[b % 3]])

    def st_M1(b):
        ex, ic, wc, sh = blocks[b]
        gu = PS[1 + b % 2]
        for k in range(8):
            if sh is None:
                lh = XT[b % 3][:, k, :]; rd_ = [XT[b % 3]]
            else:
                lh = h2T[:, k, sh * 128:(sh + 1) * 128]; rd_ = [(h2T, sh)]
            S.op(PE, lambda e: e.matmul(gu[:, 0:512], lhsT=lh, rhs=wgu[ex % NWS][:, k, :], start=(k == 0), stop=(k == 7)),
                 rd_ + [wgu[ex % NWS]], [gu])
        sg_ = sgm[b % 2]; tg_ = tg[b % 2]; a_ = Ab[b % 2]
        S.op(ACT, lambda e: e.activation(out=sg_[:, :], in_=gu[:, 0:256], func=AF.Sigmoid), [gu], [sg_])
        S.op(DVE, lambda e: e.tensor_tensor(out=tg_[:, :], in0=gu[:, 0:256], in1=sg_[:, :], op=ALU.mult), [gu, sg_], [tg_])
        S.op(DVE, lambda e: e.scalar_tensor_tensor(out=a_[:, :], in0=tg_[:, :], scalar=wc, in1=gu[:, 256:512],
                                                   op0=ALU.mult, op1=ALU.mult), [gu, tg_, wT, onesf], [a_])

    def st_M2(b):
        ex, ic, wc, sh = blocks[b]
        a_ = Ab[b % 2]; at_ = ATb[b % 2]; y_ = Yb[b % 2]
        for c in range(2):
            a_in = a_[:, c * 128:(c + 1) * 128] if sh is not None else a_[:, :].rearrange("s (p c) -> s c p", c=2)[:, c, :]
            S.op(PE, lambda e: e.transpose(out=PSB[5][:, c * 128:(c + 1) * 128], in_=a_in,
                                           identity=identb[:, :]), [a_, identb], [PS[5]])
        S.op(ACT, lambda e: e.copy(out=at_[:, :, :], in_=cols(PSB[5][:, 0:256], 2)), [PS[5]], [at_])
        for hh in range(2):
            for c in range(2):
                S.op(PE, lambda e: e.matmul(PS[6 + hh][:, 0:512], lhsT=at_[:, c, :], rhs=wdn[ex % NWS][:, c, hh * 512:(hh + 1) * 512],
                                            start=(c == 0), stop=(c == 1)), [at_, wdn[ex % NWS]], [PS[6 + hh]])
        S.op(ACT, lambda e: e.copy(out=y_[:, 0:512], in_=PS[6][:, 0:512]), [PS[6]], [y_])
        S.op(DVE, lambda e: e.tensor_copy(out=y_[:, 512:1024], in_=PS[7][:, 0:512]), [PS[7]], [y_])
        S.dma(POOL, lambda e: e.indirect_dma_start(out=ACCd[b % NACC][:, :], out_offset=bass.IndirectOffsetOnAxis(ap=ic, axis=0),
                                                   in_=y_[:, :], in_offset=None, bounds_check=bc_reg(e), oob_is_err=False,
                                                   compute_op=ALU.add),
              [y_, idxT, iot], [ACCb[b % NACC]])

    stage_expert(0); stage_expert(1)
    cast_expert(0)
    stage_expert(2)
    cast_expert(1)
    for b in range(3):
        st_gather(b)
    st_T(0); st_T(1)
    for b in range(NB):
        ex = blocks[b][0]
        if b + 3 < NB:
            st_gather(b + 3)
        st_M1(b)
        if b + 2 < NB:
            st_T(b + 2)
        if b >= 1:
            st_M2(b - 1)
        if b == 0 or blocks[b - 1][0] != ex:
            if ex + 2 <= 256:
                cast_expert(ex + 2)
            if ex + 3 <= 256:
                stage_expert(ex + 3)
    st_M2(NB - 1)
    A.release(m4)
    S.barrier()
    xf = [A.alloc([128, 1024], F32, f"xf{i}") for i in range(2)]
    af = [[A.alloc([128, 1024], F32, f"af{i}{a}") for a in range(NACC)] for i in range(2)]
    fj = A.alloc([128, 1024], BF16, "fj")
    fss = [A.alloc([128, 1], F32, f"fss{i}") for i in range(2)]; frs = [A.alloc([128, 1], F32, f"frs{i}") for i in range(2)]
    for it in range(NT):
        x_ = xf[it % 2]; ss_ = fss[it % 2]; rs_ = frs[it % 2]; af_ = af[it % 2]
        S.dma(SP, lambda e: e.dma_start(out=x_[:, :], in_=X1[it * 128:(it + 1) * 128, :]), [(X1b, it)], [x_])
        for a in range(NACC):
            S.dma(SP, lambda e: e.dma_start(out=af_[a][:, :], in_=ACCd[a][it * 128:(it + 1) * 128, :]), [ACCb[a]], [af_[a]])
        S.op(DVE, lambda e: e.tensor_tensor(out=af_[0][:, :], in0=af_[0][:, :], in1=af_[1][:, :], op=ALU.add), [af_[0], af_[1]], [af_[0]])
        S.op(DVE, lambda e: e.tensor_tensor(out=af_[2][:, :], in0=af_[2][:, :], in1=af_[3][:, :], op=ALU.add), [af_[2], af_[3]], [af_[2]])
        S.op(DVE, lambda e: e.tensor_tensor(out=af_[0][:, :], in0=af_[0][:, :], in1=af_[2][:, :], op=ALU.add), [af_[0], af_[2]], [af_[0]])
        S.op(DVE, lambda e: e.tensor_tensor(out=af_[0][:, :], in0=af_[0][:, :], in1=mod[:, 5, :], op=ALU.mult), [af_[0], (mod, 5)], [af_[0]])
        S.op(DVE, lambda e: e.tensor_tensor(out=x_[:, :], in0=x_[:, :], in1=af_[0][:, :], op=ALU.add), [x_, af_[0]], [x_])
        rstd_of(ss_, rs_, x_[:, :], x_, fj)
        S.op(DVE, lambda e: e.scalar_tensor_tensor(out=x_[:, :], in0=x_[:, :], scalar=rs_[:, 0:1], in1=fgt[:, :],
                                                   op0=ALU.mult, op1=ALU.mult), [x_, rs_, fgt], [x_])
        S.dma(SP, lambda e: e.dma_start(out=out_d[it * 128:(it + 1) * 128, :], in_=x_[:, :]), [x_], [])
    S.analyze()
    S.emit()
    return nc


_CONST_CACHE = {}


def _constants():
    if _CONST_CACHE:
        return _CONST_CACHE
    ident = np.eye(128, dtype=np.float32)
    tri = (np.arange(128)[:, None] <= np.arange(128)[None, :]).astype(np.float32)
    H = 8
    gamma = 1.0 - np.exp2(-5.0 - np.arange(H, dtype=np.float64))
    half = 32
    inv_freq = (np.float32(10000.0) ** (-(np.arange(half, dtype=np.float32) / np.float32(half)))).astype(np.float32)
    pos = np.arange(4096, dtype=np.float32)
    ang = (pos[:, None] * inv_freq[None, :]).astype(np.float32).astype(np.float64)
    cos, sin = np.cos(ang), np.sin(ang)
    p = (np.arange(4096) % 128).astype(np.float64)
    dq = gamma[None, :] ** (p[:, None] + 1.0)
    dk = gamma[None, :] ** (-(p[:, None] + 1.0)) * (64 ** -0.5)
    def tab(cs, d):
        return (cs[:, None, :] * d[:, :, None]).reshape(4096, 256).astype(np.float32)
    _CONST_CACHE.update(
        ident=ident, tri=tri,
        k_c=tab(cos, dk), k_s=tab(sin, dk), q_c=tab(cos, dq), q_s=tab(sin, dq))
    cd = gamma ** 128.0
    cdt = np.zeros((128, 4, 128), np.float32)
    for m in range(4):
        cdt[0:64, m, :] = cd[2 * m]
        cdt[64:128, m, :] = cd[2 * m + 1]
    _CONST_CACHE["cdt"] = cdt.reshape(128, 512)
    return _CONST_CACHE


_NC_CACHE = {}


def kernel(x, c, w_ada, b_ada, norm1_g, w_in, b_forget, ret_gn_g, w_branch_a, w_branch_b, w_out, norm2_g,
           w_router, router_bias, w_exp_gate, w_exp_up, w_exp_down, w_sh_gate, w_sh_up, w_sh_down, final_g):
    f32 = lambda a: np.ascontiguousarray(np.asarray(a, dtype=np.float32))
    x = f32(x); c = f32(c)
    K = _constants()
    bc = lambda v: np.ascontiguousarray(np.broadcast_to(f32(v).reshape(1, -1), (128, f32(v).size)))
    shared = {
        "w_ada": f32(w_ada)[0], "b_ada_bc": bc(f32(b_ada)[0]),
        "g1_bc": bc(f32(norm1_g)[0]), "g2_bc": bc(f32(norm2_g)[0]), "fg_bc": bc(f32(final_g)),
        "w_in": f32(w_in)[0], "bfg_bc": bc(f32(b_forget)[0]),
        "gng": np.ascontiguousarray(f32(ret_gn_g)[0].reshape(8, 128).T),
        "w_ba": f32(w_branch_a)[0], "w_bb": f32(w_branch_b)[0], "w_out": f32(w_out)[0],
        "w_router": f32(w_router)[0], "rbias_bc": bc(f32(router_bias)[0]),
        "w_eg": f32(w_exp_gate)[0], "w_eu": f32(w_exp_up)[0], "w_ed": f32(w_exp_down)[0],
        "w_sg": f32(w_sh_gate)[0], "w_su": f32(w_sh_up)[0], "w_sd": f32(w_sh_down)[0],
        "ident": K["ident"], "tri": K["tri"], "cdt": K["cdt"],
        "iota": (np.arange(16, dtype=np.int32)[None, :] * 128 + np.arange(128, dtype=np.int32)[:, None]).astype(np.int32),
        "dummyrow": np.ascontiguousarray(np.broadcast_to((2048.0 + (np.arange(512) % 128)).astype(np.float32)[None, :], (128, 512))),
    }
    in_maps = []
    for core in range(8):
        b, half = core // 2, core % 2
        lo = half * 2048
        m = dict(shared)
        m["xo"] = np.ascontiguousarray(x[b, lo:lo + 2048])
        m["xc"] = np.ascontiguousarray(x[b, 0:2048])
        m["cin"] = np.ascontiguousarray(c[b].reshape(8, 128).T)
        m["flag"] = np.full((128, 1), float(half), np.float32)
        m["rkc_c"] = K["k_c"][0:2048]; m["rkc_s"] = K["k_s"][0:2048]
        m["rko_c"] = K["k_c"][lo:lo + 2048]; m["rko_s"] = K["k_s"][lo:lo + 2048]
        m["rqo_c"] = K["q_c"][lo:lo + 2048]; m["rqo_s"] = K["q_s"][lo:lo + 2048]
        in_maps.append(m)
    if "nc" not in _NC_CACHE:
        _NC_CACHE["nc"] = build_program()
    res = run_bass_kernel_spmd(_NC_CACHE["nc"], in_maps, core_ids=list(range(8)))
    out = np.empty((4, 4096, 1024), np.float32)
    for core in range(8):
        b, half = core // 2, core % 2
        out[b, half * 2048:(half + 1) * 2048] = res.results[core]["out"]
    return out
```
